# Optimizing a Trainium2 kernel written in Bass

```python
import math
import jax
import jax.numpy as jnp
from jax import lax
import numpy as np

D_MODEL = 1024
BATCH = 4
SEQ = 8192
DEPTH = 2

GRID_W = 64
CTX_LEN = 256
EPS = 1e-6
ROPE_BASE = 10000.0
Q_BLOCK = 128
F32 = jnp.float32

ML_HEADS = 4
ML_DH = 64
ML_W = ML_HEADS * ML_DH
ML_CHUNK = 128
ML_COLS = 4 * ML_W + 4 * ML_HEADS

MLA_HEADS = 4
MLA_Q_RANK = 192
MLA_KV_RANK = 128
MLA_NOPE = 64
MLA_ROPE = 32
MLA_V = 64
MLA_W = MLA_HEADS * MLA_V
MLA_COLS = MLA_Q_RANK + MLA_KV_RANK + MLA_ROPE

SW_HEADS = 4
SW_KV_HEADS = 2
SW_DH = 64
SW_WINDOW = 128
SW_BLOCK = 128
SW_W = SW_HEADS * SW_DH
SW_COLS = (SW_HEADS + 2 * SW_KV_HEADS) * SW_DH

LRU_W = 256
LRU_BLOCKS = 4
LRU_BW = LRU_W // LRU_BLOCKS
LRU_CONV = 4
LRU_C = 8.0
LRU_COLS = 2 * LRU_W

D_IN = ML_COLS + MLA_COLS + SW_COLS + LRU_COLS
D_MIX = ML_W + MLA_W + SW_W + LRU_W
COL_SPLITS = (ML_COLS, ML_COLS + MLA_COLS, ML_COLS + MLA_COLS + SW_COLS)

D_FF = 2816
N_EXPERTS = 8
TOP_K = 2
D_FF_EXPERT = 3584

kernel_name = 'hybrid_mlstm_mla_swa_rglru_moe_dit'


def rmsnorm(x, g):
    xf = x.astype(F32)
    y = xf * lax.rsqrt(jnp.mean(xf * xf, axis=-1, keepdims=True) + EPS)
    return (y * g.astype(F32)).astype(x.dtype)


def modulate(h, shift, scale):
    return h * (1.0 + scale) + shift


def _rotate(x, pos):
    nf = x.shape[-1] // 2
    freqs = ROPE_BASE ** (-jnp.arange(nf, dtype=F32) / nf)
    ang = pos.astype(F32)[:, None] * freqs[None, :]
    cos, sin = jnp.cos(ang), jnp.sin(ang)
    x1 = x[..., :nf].astype(F32)
    x2 = x[..., nf:].astype(F32)
    return jnp.concatenate([x1 * cos - x2 * sin, x1 * sin + x2 * cos], axis=-1)


def rope_2d(x, rows, cols):
    half = x.shape[-1] // 2
    y = jnp.concatenate([_rotate(x[..., :half], rows), _rotate(x[..., half:], cols)], axis=-1)
    return y.astype(x.dtype)


def softmax_with_sink(s, sink):
    m = jnp.maximum(jnp.max(s, axis=-1, keepdims=True), sink)
    e = jnp.exp(s - m)
    return e / (jnp.sum(e, axis=-1, keepdims=True) + jnp.exp(sink - m))


def block_attention(q, k, v):
    B, H, T, dq = q.shape
    nb = T // Q_BLOCK
    scale = dq ** -0.5
    qb = jnp.moveaxis(q.reshape(B, H, nb, Q_BLOCK, dq), 2, 0)

    def one_block(qblk):
        s = jnp.einsum('bhqd,bhkd->bhqk', qblk, k).astype(F32) * scale
        p = jax.nn.softmax(s, axis=-1)
        return jnp.einsum('bhqk,bhkd->bhqd', p.astype(v.dtype), v)

    o = lax.map(one_block, qb)
    return jnp.moveaxis(o, 0, 2).reshape(B, H, T, v.shape[-1])


def mlstm_chunked(q, k, v, ig, fg, state, with_out):
    B, H, T, d = k.shape
    nc = T // ML_CHUNK

    def chunks(a):
        return jnp.moveaxis(a.astype(F32).reshape(B, H, nc, ML_CHUNK, *a.shape[3:]), 2, 0)

    xs = (chunks(k), chunks(v), chunks(ig), chunks(jax.nn.log_sigmoid(fg.astype(F32))))
    if with_out:
        xs = xs + (chunks(q),)
    causal = jnp.tril(jnp.ones((ML_CHUNK, ML_CHUNK), dtype=bool))

    def step(carry, blk):
        C, n, m = carry
        kc, vc, ic, lfc = blk[:4]
        b = jnp.cumsum(lfc, axis=-1)
        h = None
        if with_out:
            qc = blk[4]
            d_mat = jnp.where(causal, b[..., :, None] - b[..., None, :] + ic[..., None, :], -jnp.inf)
            m_inter = b + m[..., None]
            m_t = jnp.maximum(m_inter, jnp.max(d_mat, axis=-1))
            w_inter = jnp.exp(m_inter - m_t)
            s = jnp.einsum('bhtd,bhsd->bhts', qc, kc) * jnp.exp(d_mat - m_t[..., None])
            num = jnp.einsum('bhts,bhsd->bhtd', s, vc) + w_inter[..., None] * jnp.einsum('bhvk,bhtk->bhtv', C, qc)
            den = jnp.sum(s, axis=-1) + w_inter * jnp.einsum('bhk,bhtk->bht', n, qc)
            h = num / jnp.maximum(jnp.abs(den), jnp.exp(-m_t))[..., None]
        b_last = b[..., -1]
        g = b_last[..., None] - b + ic
        m_new = jnp.maximum(b_last + m, jnp.max(g, axis=-1))
        w = jnp.exp(g - m_new[..., None])
        decay = jnp.exp(b_last + m - m_new)
        C = decay[..., None, None] * C + jnp.einsum('bhs,bhsv,bhsk->bhvk', w, vc, kc)
        n = decay[..., None] * n + jnp.einsum('bhs,bhsk->bhk', w, kc)
        return (C, n, m_new), h

    state, hs = lax.scan(step, state, xs)
    if with_out:
        hs = jnp.moveaxis(hs, 0, 2).reshape(B, H, T, d)
    return hs, state


def mlstm_mixer(zc, zl, gate_b, out_gain, ctx_out):
    def split_heads(a):
        B, T, _ = a.shape
        return a.reshape(B, T, ML_HEADS, ML_DH).transpose(0, 2, 1, 3)

    def prep(z):
        q, k, v, o, g = jnp.split(z, [ML_W, 2 * ML_W, 3 * ML_W, 4 * ML_W], axis=-1)
        g = (g.astype(F32) + gate_b.astype(F32)).transpose(0, 2, 1)
        gates = jnp.split(g, 4, axis=1)
        return split_heads(q), split_heads(k) * (ML_DH ** -0.5), split_heads(v), o, gates

    def flip(a):
        return jnp.flip(a, axis=2)

    qc, kc, vc, oc, gc = prep(zc)
    ql, kl, vl, ol, gl = prep(zl)
    B = zl.shape[0]
    zero = (jnp.zeros((B, ML_HEADS, ML_DH, ML_DH), F32), jnp.zeros((B, ML_HEADS, ML_DH), F32),
            jnp.zeros((B, ML_HEADS), F32))
    hc_f, st_f = mlstm_chunked(qc if ctx_out else None, kc, vc, gc[0], gc[1], zero, ctx_out)
    hc_b, st_b = mlstm_chunked(flip(qc) if ctx_out else None, flip(kc), flip(vc), flip(gc[2]), flip(gc[3]), zero, ctx_out)
    hl_f, _ = mlstm_chunked(ql, kl, vl, gl[0], gl[1], st_f, True)
    hl_b, _ = mlstm_chunked(flip(ql), flip(kl), flip(vl), flip(gl[2]), flip(gl[3]), st_b, True)
    g_norm = out_gain.reshape(ML_HEADS, 1, ML_DH)

    def finish(h_f, h_b, o):
        B_, H, T, d = h_f.shape
        h = rmsnorm(h_f + flip(h_b), g_norm).transpose(0, 2, 1, 3).reshape(B_, T, ML_W)
        return (jax.nn.sigmoid(o.astype(F32)) * h).astype(zl.dtype)

    yl = finish(hl_f, hl_b, ol)
    yc = finish(hc_f, hc_b, oc) if ctx_out else None
    return yc, yl


def mla_mixer(zc, zl, q_norm, w_uq, kv_norm, w_ukv, q_gain, k_gain, rows, cols, ctx_out):
    def project(z, with_q, rope):
        B, T, _ = z.shape
        cq, ckv, kr = jnp.split(z, [MLA_Q_RANK, MLA_Q_RANK + MLA_KV_RANK], axis=-1)
        kv = (rmsnorm(ckv, kv_norm) @ w_ukv).reshape(B, T, MLA_HEADS, MLA_NOPE + MLA_V).transpose(0, 2, 1, 3)
        k_nope = rmsnorm(kv[..., :MLA_NOPE], k_gain[:MLA_NOPE])
        v = kv[..., MLA_NOPE:]
        k_rope = rmsnorm(kr, k_gain[MLA_NOPE:])[:, None]
        if rope:
            k_rope = rope_2d(k_rope, rows, cols)
        k = jnp.concatenate([k_nope, jnp.broadcast_to(k_rope, (B, MLA_HEADS, T, MLA_ROPE))], axis=-1)
        q = None
        if with_q:
            qf = (rmsnorm(cq, q_norm) @ w_uq).reshape(B, T, MLA_HEADS, MLA_NOPE + MLA_ROPE).transpose(0, 2, 1, 3)
            q_nope = rmsnorm(qf[..., :MLA_NOPE], q_gain[:MLA_NOPE])
            q_rope = rmsnorm(qf[..., MLA_NOPE:], q_gain[MLA_NOPE:])
            if rope:
                q_rope = rope_2d(q_rope, rows, cols)
            q = jnp.concatenate([q_nope, q_rope], axis=-1)
        return q, k, v

    def merge(o):
        return o.transpose(0, 2, 1, 3).reshape(o.shape[0], o.shape[2], MLA_W)

    qc, kc, vc = project(zc, ctx_out, False)
    ql, kl, vl = project(zl, True, True)
    yl = merge(block_attention(ql, jnp.concatenate([kl, kc], axis=2), jnp.concatenate([vl, vc], axis=2)))
    yc = merge(block_attention(qc, kc, vc)) if ctx_out else None
    return yc, yl


def swa_mixer(zc, zl, q_gain, k_gain, sink, rows, cols, ctx_out):
    G = SW_HEADS // SW_KV_HEADS

    def project(z, with_q, rope):
        B, T, _ = z.shape
        q, k, v = jnp.split(z, [SW_W, SW_W + SW_KV_HEADS * SW_DH], axis=-1)
        k = rmsnorm(k.reshape(B, T, SW_KV_HEADS, SW_DH).transpose(0, 2, 1, 3), k_gain)
        v = v.reshape(B, T, SW_KV_HEADS, SW_DH).transpose(0, 2, 1, 3)
        if rope:
            k = rope_2d(k, rows, cols)
        if with_q:
            q = rmsnorm(q.reshape(B, T, SW_KV_HEADS, G, SW_DH).transpose(0, 2, 3, 1, 4), q_gain)
            if rope:
                q = rope_2d(q, rows, cols)
        else:
            q = None
        return q, k, v

    scale = SW_DH ** -0.5
    sink_h = sink.astype(F32).reshape(SW_KV_HEADS, G)
    qc, kc, vc = project(zc, ctx_out, False)
    ql, kl, vl = project(zl, True, True)
    B, T = zl.shape[0], zl.shape[1]
    nb = T // SW_BLOCK
    KL = SW_BLOCK + 2 * SW_WINDOW
    idx = jnp.arange(nb)[:, None] * SW_BLOCK + jnp.arange(KL)[None, :]
    pad = ((0, 0), (0, 0), (SW_WINDOW, SW_WINDOW), (0, 0))
    kb = jnp.pad(kl, pad)[:, :, idx]
    vb = jnp.pad(vl, pad)[:, :, idx]
    qb = ql.reshape(B, SW_KV_HEADS, G, nb, SW_BLOCK, SW_DH)
    s_loc = jnp.einsum('bhgnqd,bhnkd->bhgnqk', qb, kb).astype(F32) * scale
    s_ctx = jnp.einsum('bhgnqd,bhkd->bhgnqk', qb, kc).astype(F32) * scale
    qpos = jnp.arange(nb)[:, None, None] * SW_BLOCK + jnp.arange(SW_BLOCK)[None, :, None]
    kpos = jnp.arange(nb)[:, None, None] * SW_BLOCK + jnp.arange(KL)[None, None, :] - SW_WINDOW
    valid = (kpos >= 0) & (kpos < T) & (jnp.abs(qpos - kpos) <= SW_WINDOW)
    s = jnp.concatenate([jnp.where(valid, s_loc, -jnp.inf), s_ctx], axis=-1)
    p = softmax_with_sink(s, sink_h[:, :, None, None, None]).astype(vl.dtype)
    o = (jnp.einsum('bhgnqk,bhnkd->bhgnqd', p[..., :KL], vb)
         + jnp.einsum('bhgnqk,bhkd->bhgnqd', p[..., KL:], vc))
    yl = o.reshape(B, SW_KV_HEADS, G, T, SW_DH).transpose(0, 3, 1, 2, 4).reshape(B, T, SW_W)
    yc = None
    if ctx_out:
        sc = jnp.einsum('bhgqd,bhkd->bhgqk', qc, kc).astype(F32) * scale
        pc = softmax_with_sink(sc, sink_h[:, :, None, None]).astype(vc.dtype)
        oc = jnp.einsum('bhgqk,bhkd->bhgqd', pc, vc)
        yc = oc.transpose(0, 3, 1, 2, 4).reshape(zc.shape[0], zc.shape[1], SW_W)
    return yc, yl


def conv_centred(u, w, b):
    C = u.shape[-1]
    y = lax.conv_general_dilated(u, w[:, None, :].astype(u.dtype), window_strides=(1,),
                                 padding=[(LRU_CONV // 2, LRU_CONV - 1 - LRU_CONV // 2)],
                                 dimension_numbers=('NWC', 'WIO', 'NWC'), feature_group_count=C)
    return y + b.astype(u.dtype)


def rglru_coeffs(u, wa, ba, wx, bx, lam):
    B, T, _ = u.shape
    uf = u.astype(F32)
    ub = uf.reshape(B, T, LRU_BLOCKS, LRU_BW)
    r = jax.nn.sigmoid(jnp.einsum('btnc,ncd->btnd', ub, wa.astype(F32)).reshape(B, T, LRU_W) + ba)
    i = jax.nn.sigmoid(jnp.einsum('btnc,ncd->btnd', ub, wx.astype(F32)).reshape(B, T, LRU_W) + bx)
    log_a = -LRU_C * r * jax.nn.softplus(-lam.astype(F32))
    a = jnp.exp(log_a)
    b = jnp.sqrt(-jnp.expm1(2.0 * log_a)) * (i * uf)
    return a, b


def linear_scan(a, b, h0):
    def combine(l, r):
        return l[0] * r[0], r[0] * l[1] + r[1]
    a_cum, h = lax.associative_scan(combine, (a, b), axis=1)
    h = h + a_cum * h0[:, None, :]
    return h, h[:, -1]


def rglru_mixer(zc, zl, conv_w, conv_b, wa, ba, wx, bx, lam, ctx_out):
    uc_in, gc = jnp.split(zc, 2, axis=-1)
    ul_in, gl = jnp.split(zl, 2, axis=-1)
    uc = conv_centred(uc_in, conv_w, conv_b)
    ul = conv_centred(ul_in, conv_w, conv_b)
    zero = jnp.zeros((zl.shape[0], LRU_W), F32)

    def flip(a):
        return jnp.flip(a, axis=1)

    def run(d, u_ctx, u_lat):
        hc, hc_last = linear_scan(*rglru_coeffs(u_ctx, wa[d], ba[d], wx[d], bx[d], lam[d]), zero)
        hl, _ = linear_scan(*rglru_coeffs(u_lat, wa[d], ba[d], wx[d], bx[d], lam[d]), hc_last)
        return hc, hl

    hc_f, hl_f = run(0, uc, ul)
    hc_b, hl_b = run(1, flip(uc), flip(ul))

    def finish(h_f, h_b, g):
        return (jax.nn.gelu(g.astype(F32)) * (h_f + flip(h_b))).astype(zl.dtype)

    yl = finish(hl_f, hl_b, gl)
    yc = finish(hc_f, hc_b, gc) if ctx_out else None
    return yc, yl


def swiglu(h, w13, w2):
    a, g = jnp.split(h @ w13, 2, axis=-1)
    return (jax.nn.silu(g) * a) @ w2


def moe_ffn(h, router, router_b, w13, w2):
    B, T, D = h.shape
    hf = h.reshape(B * T, D)
    logits = (hf @ router).astype(F32) + router_b.astype(F32)
    top_val, top_idx = lax.top_k(logits, TOP_K)
    gates = jax.nn.softmax(top_val, axis=-1)
    combine = jnp.einsum('nk,nke->ne', gates, jax.nn.one_hot(top_idx, N_EXPERTS, dtype=F32))
    out = jnp.zeros_like(hf)
    for e in range(N_EXPERTS):
        out = out + combine[:, e:e + 1].astype(hf.dtype) * swiglu(hf, w13[e], w2[e])
    return out.reshape(B, T, D)


def setup_inputs(seed: int = 0) -> dict:
    key = jax.random.key(seed)
    ks = iter(jax.random.split(key, 48))
    D = D_MODEL
    n_dense = (DEPTH + 1) // 2
    n_moe = DEPTH // 2

    def nrm(shape, scale):
        return jax.random.normal(next(ks), shape, F32) * scale

    def gain(shape):
        return 1.0 + nrm(shape, 0.05)

    i_b = nrm((DEPTH, 2, ML_HEADS), 0.1)
    f_b = 3.0 + 3.0 * jax.random.uniform(next(ks), (DEPTH, 2, ML_HEADS), F32)
    ml_gate_b = jnp.stack([i_b[:, 0], f_b[:, 0], i_b[:, 1], f_b[:, 1]], axis=1).reshape(DEPTH, 4 * ML_HEADS)
    u = jax.random.uniform(next(ks), (DEPTH, 2, LRU_W), F32, minval=0.9, maxval=0.999)
    lru_lam = jnp.log(u) - jnp.log1p(-u)
    return {
        'x': nrm((BATCH, SEQ, D), 1.0),
        'c': nrm((BATCH, D), 1.0),
        'ctx': nrm((BATCH, CTX_LEN, D), 1.0),
        'c_ctx': nrm((D,), 1.0),
        'ada_w': nrm((DEPTH, D, 6 * D), 0.5 * D ** -0.5),
        'ada_b': nrm((DEPTH, 6 * D), 0.02),
        'norm_mix': gain((DEPTH, D)),
        'norm_ffn': gain((DEPTH, D)),
        'w_in': nrm((DEPTH, D, D_IN), D ** -0.5),
        'w_out': nrm((DEPTH, D_MIX, D), D_MIX ** -0.5),
        'ml_gate_b': ml_gate_b,
        'ml_out_norm': gain((DEPTH, ML_W)),
        'mla_q_norm': gain((DEPTH, MLA_Q_RANK)),
        'mla_w_uq': nrm((DEPTH, MLA_Q_RANK, MLA_HEADS * (MLA_NOPE + MLA_ROPE)), MLA_Q_RANK ** -0.5),
        'mla_kv_norm': gain((DEPTH, MLA_KV_RANK)),
        'mla_w_ukv': nrm((DEPTH, MLA_KV_RANK, MLA_HEADS * (MLA_NOPE + MLA_V)), MLA_KV_RANK ** -0.5),
        'mla_q_gain': gain((DEPTH, MLA_NOPE + MLA_ROPE)),
        'mla_k_gain': gain((DEPTH, MLA_NOPE + MLA_ROPE)),
        'sw_q_gain': gain((DEPTH, SW_DH)),
        'sw_k_gain': gain((DEPTH, SW_DH)),
        'sw_sink': nrm((DEPTH, SW_HEADS), 0.5),
        'lru_conv_w': nrm((DEPTH, LRU_CONV, LRU_W), 0.5),
        'lru_conv_b': nrm((DEPTH, LRU_W), 0.02),
        'lru_wa': nrm((DEPTH, 2, LRU_BLOCKS, LRU_BW, LRU_BW), LRU_BW ** -0.5),
        'lru_ba': nrm((DEPTH, 2, LRU_W), 0.02),
        'lru_wx': nrm((DEPTH, 2, LRU_BLOCKS, LRU_BW, LRU_BW), LRU_BW ** -0.5),
        'lru_bx': nrm((DEPTH, 2, LRU_W), 0.02),
        'lru_lam': lru_lam,
        'ffn_w13': nrm((n_dense, D, 2 * D_FF), D ** -0.5),
        'ffn_w2': nrm((n_dense, D_FF, D), D_FF ** -0.5),
        'moe_router': nrm((n_moe, D, N_EXPERTS), D ** -0.5),
        'moe_router_b': nrm((n_moe, N_EXPERTS), 0.01),
        'moe_w13': nrm((n_moe, N_EXPERTS, D, 2 * D_FF_EXPERT), D ** -0.5),
        'moe_w2': nrm((n_moe, N_EXPERTS, D_FF_EXPERT, D), D_FF_EXPERT ** -0.5),
    }


def reference(x, c, ctx, c_ctx, ada_w, ada_b, norm_mix, norm_ffn, w_in, w_out, ml_gate_b, ml_out_norm,
              mla_q_norm, mla_w_uq, mla_kv_norm, mla_w_ukv, mla_q_gain, mla_k_gain, sw_q_gain, sw_k_gain,
              sw_sink, lru_conv_w, lru_conv_b, lru_wa, lru_ba, lru_wx, lru_bx, lru_lam, ffn_w13, ffn_w2,
              moe_router, moe_router_b, moe_w13, moe_w2):
    T = x.shape[1]
    ROWS = T // GRID_W
    rows = jnp.repeat(jnp.arange(ROWS, dtype=jnp.int32), GRID_W)
    cols = jnp.tile(jnp.arange(GRID_W, dtype=jnp.int32), ROWS)
    xl, xc = x, ctx
    for l in range(DEPTH):
        ctx_out = l < DEPTH - 1
        mod_l = (jax.nn.silu(c) @ ada_w[l] + ada_b[l])[:, None, :]
        mod_c = jax.nn.silu(c_ctx) @ ada_w[l] + ada_b[l]
        sh1, sc1, g1, sh2, sc2, g2 = jnp.split(mod_l, 6, axis=-1)
        csh1, csc1, cg1, csh2, csc2, cg2 = jnp.split(mod_c, 6, axis=-1)

        hl = modulate(rmsnorm(xl, norm_mix[l]), sh1, sc1)
        hc = modulate(rmsnorm(xc, norm_mix[l]), csh1, csc1)
        za, zb, zc_, zd = jnp.split(hl @ w_in[l], COL_SPLITS, axis=-1)
        ca, cb, cc, cd = jnp.split(hc @ w_in[l], COL_SPLITS, axis=-1)
        ya_c, ya_l = mlstm_mixer(ca, za, ml_gate_b[l], ml_out_norm[l], ctx_out)
        yb_c, yb_l = mla_mixer(cb, zb, mla_q_norm[l], mla_w_uq[l], mla_kv_norm[l], mla_w_ukv[l],
                               mla_q_gain[l], mla_k_gain[l], rows, cols, ctx_out)
        yc_c, yc_l = swa_mixer(cc, zc_, sw_q_gain[l], sw_k_gain[l], sw_sink[l], rows, cols, ctx_out)
        yd_c, yd_l = rglru_mixer(cd, zd, lru_conv_w[l], lru_conv_b[l], lru_wa[l], lru_ba[l], lru_wx[l],
                                 lru_bx[l], lru_lam[l], ctx_out)
        yl = jnp.concatenate([ya_l, yb_l, yc_l, yd_l], axis=-1) @ w_out[l]
        xl = xl + g1 * yl

        hl = modulate(rmsnorm(xl, norm_ffn[l]), sh2, sc2)
        if l % 2 == 0:
            xl = xl + g2 * swiglu(hl, ffn_w13[l // 2], ffn_w2[l // 2])
        else:
            xl = xl + g2 * moe_ffn(hl, moe_router[l // 2], moe_router_b[l // 2], moe_w13[l // 2], moe_w2[l // 2])

        if ctx_out:
            yc = jnp.concatenate([ya_c, yb_c, yc_c, yd_c], axis=-1) @ w_out[l]
            xc = xc + cg1 * yc
            hc = modulate(rmsnorm(xc, norm_ffn[l]), csh2, csc2)
            if l % 2 == 0:
                xc = xc + cg2 * swiglu(hc, ffn_w13[l // 2], ffn_w2[l // 2])
            else:
                xc = xc + cg2 * moe_ffn(hc, moe_router[l // 2], moe_router_b[l // 2], moe_w13[l // 2], moe_w2[l // 2])
    return xl
```

```python
import contextlib
import numpy as np
import concourse.bass as bass
import concourse.mybir as mybir
from concourse.bass_utils import run_bass_kernel_spmd

F32 = mybir.dt.float32
BF16 = mybir.dt.bfloat16
I32 = mybir.dt.int32
U32 = mybir.dt.uint32
ALU = mybir.AluOpType
AF = mybir.ActivationFunctionType
AX = mybir.AxisListType

_ESZ = {F32: 4, BF16: 2, I32: 4, U32: 4}


def _esz(dt):
    return _ESZ[dt]


def _box(ap):
    t = ap.tensor
    e = _esz(ap.dtype)
    pat = ap.ap
    off = int(ap.offset)
    kind = type(t).__name__
    if kind.startswith("DRam"):
        lo = off
        hi = off
        for s, c in pat:
            d = s * (c - 1)
            if d < 0:
                lo += d
            else:
                hi += d
        return (t.name, 0, 1, lo * e, (hi + 1) * e)
    R, npart = pat[0]
    if R == 0:
        R = 1 << 30
    p0 = off // R
    fo = off % R
    lo = fo
    hi = fo
    for s, c in pat[1:]:
        d = s * (c - 1)
        if d < 0:
            lo += d
        else:
            hi += d
    if kind.startswith("PSum"):
        return (t.name, (p0 // 32) * 32, ((p0 + npart + 31) // 32) * 32, 0, 2048)
    return (t.name, p0, p0 + npart, lo * e, (hi + 1) * e)


def _ovl(a, b):
    return a[1] < b[2] and b[1] < a[2] and a[3] < b[4] and b[3] < a[4]


def _contains(a, b):
    return a[1] <= b[1] and b[2] <= a[2] and a[3] <= b[3] and b[4] <= a[4]


SIGNAL_ALL = False
PIPE_MLA = False


class Op:
    __slots__ = ("id", "eng", "fn", "deps", "signal", "dma", "idx", "ord", "dsem", "dval", "prewait")


class Sched:
    ENGS = ("pe", "dve", "act", "pool", "sp")
    MAXV = 20000
    ND = 8

    def __init__(self, nc):
        self.nc = nc
        self.ops = []
        self.eops = {e: [] for e in self.ENGS}
        self.acc = {}
        self.ndma = {e: 0 for e in self.ENGS}
        self.out_dmas = []

    def add(self, eng, fn, reads=(), writes=(), signal=True, dma=False):
        op = Op()
        op.id = len(self.ops)
        op.eng = eng
        op.fn = fn
        op.signal = signal
        op.dma = dma
        op.deps = set()
        op.idx = len(self.eops[eng])
        op.prewait = None
        self.ops.append(op)
        self.eops[eng].append(op)
        rb = [_box(a) for a in reads if a is not None]
        wb = [_box(a) for a in writes if a is not None]
        for b in rb:
            lst = self.acc.setdefault(b[0], [])
            for (ob, oid, ow) in lst:
                if ow and _ovl(ob, b):
                    self._dep(op, oid, "raw")
        for b in wb:
            lst = self.acc.setdefault(b[0], [])
            for (ob, oid, ow) in lst:
                if _ovl(ob, b):
                    self._dep(op, oid, "waw" if ow else "war")
        for b in wb:
            lst = self.acc[b[0]]
            lst[:] = [r for r in lst if not _contains(b, r[0])]
            lst.append((b, op.id, True))
        for b in rb:
            lst = self.acc[b[0]]
            if not dma:
                for i, (ob, oid, ow) in enumerate(lst):
                    if (not ow) and ob == b and self.ops[oid].eng == eng and not self.ops[oid].dma:
                        lst[i] = (b, op.id, False)
                        break
                else:
                    lst.append((b, op.id, False))
            else:
                lst.append((b, op.id, False))
        if dma:
            k = self.ndma[eng]
            self.ndma[eng] = k + 1
            op.dsem = (eng, k % self.ND)
            op.dval = 16 * (k // self.ND + 1)
            if k >= self.ND:
                op.prewait = (op.dsem, 16 * (k // self.ND))
        return op

    def _dep(self, op, oid, kind):
        o = self.ops[oid]
        if o.id == op.id:
            return
        if (not o.dma) and (not op.dma) and o.eng == op.eng:
            if op.eng == "pe":
                return
        if (not o.dma) and op.dma and o.eng == op.eng and kind != "raw" and op.eng != "pool":
            pass
        op.deps.add(oid)

    def E(self, eng):
        return eng

    def mm(self, out, lhsT, rhs, start=True, stop=True, signal=None):
        if signal is None or SIGNAL_ALL:
            signal = True
        return self.add("pe", lambda e: e.matmul(out, lhsT, rhs, start=start, stop=stop),
                        reads=[lhsT, rhs], writes=[out], signal=signal)

    def transpose(self, out, in_, ident):
        return self.add("pe", lambda e: e.transpose(out, in_, ident), reads=[in_, ident], writes=[out])

    def act(self, out, in_, func, bias=None, scale=1.0, accum_out=None, eng="act"):
        kw = {}
        rd = [in_]
        if bias is not None:
            kw["bias"] = bias
            if not isinstance(bias, (int, float)):
                rd.append(bias)
        if not isinstance(scale, (int, float)):
            rd.append(scale)
        kw["scale"] = scale
        wr = [out]
        if accum_out is not None:
            kw["accum_out"] = accum_out
            wr.append(accum_out)
        return self.add(eng, lambda e: e.activation(out, in_, func, **kw), reads=rd, writes=wr)

    def tt(self, out, in0, in1, op, eng="dve"):
        return self.add(eng, lambda e: e.tensor_tensor(out, in0, in1, op), reads=[in0, in1], writes=[out])

    def ts(self, out, in0, s1, s2, op0, op1=None, eng="dve", accum_out=None):
        rd = [in0]
        if not isinstance(s1, (int, float)):
            rd.append(s1)
        if s2 is not None and not isinstance(s2, (int, float)):
            rd.append(s2)
        wr = [out]
        kw = {}
        if accum_out is not None:
            kw["accum_out"] = accum_out
            wr.append(accum_out)
        if op1 is None:
            return self.add(eng, lambda e: e.tensor_scalar(out, in0, s1, None, op0, **kw), reads=rd, writes=wr)
        return self.add(eng, lambda e: e.tensor_scalar(out, in0, s1, s2, op0, op1, **kw), reads=rd, writes=wr)

    def stt(self, out, in0, scalar, in1, op0, op1, eng="dve"):
        rd = [in0, in1]
        if not isinstance(scalar, (int, float)):
            rd.append(scalar)
        return self.add(eng, lambda e: e.scalar_tensor_tensor(out, in0, scalar, in1, op0, op1), reads=rd, writes=[out])

    def copy(self, out, in_, eng="dve"):
        if eng == "act":
            return self.act(out, in_, AF.Copy)
        return self.add(eng, lambda e: e.tensor_copy(out, in_), reads=[in_], writes=[out])

    def memset(self, out, val, eng="dve"):
        return self.add(eng, lambda e: e.memset(out, val), writes=[out])

    def reduce(self, out, in_, op, axis=AX.X, eng="dve"):
        return self.add(eng, lambda e: e.tensor_reduce(out, in_, axis, op), reads=[in_], writes=[out])

    def recip(self, out, in_):
        return self.add("dve", lambda e: e.reciprocal(out, in_), reads=[in_], writes=[out])

    def scan(self, out, d0, d1, init, op0, op1):
        rd = [d0, d1]
        if not isinstance(init, (int, float)):
            rd.append(init)
        return self.add("dve", lambda e: e.tensor_tensor_scan(out, d0, d1, init, op0, op1), reads=rd, writes=[out])

    def dma(self, out, in_, q="sp", is_output=False, **kw):
        op = self.add(q, lambda e: e.dma_start(out=out, in_=in_, **kw), reads=[in_], writes=[out], dma=True)
        if is_output:
            self.out_dmas.append(op)
        return op

    def emit(self):
        nc = self.nc
        ENGS = self.ENGS
        for e in ENGS:
            lst = [o for o in self.eops[e] if not o.dma]
            if lst:
                lst[-1].signal = True
        nsig = {}
        for e in ENGS:
            cnt = 0
            for o in self.eops[e]:
                if o.dma:
                    o.ord = None
                    continue
                if o.signal:
                    cnt += 1
                o.ord = cnt if o.signal else None
            nsig[e] = cnt
            nxt = None
            for o in reversed(self.eops[e]):
                if o.dma:
                    continue
                if o.signal:
                    nxt = o.ord
                else:
                    o.ord = -nxt
        import contextlib
        with contextlib.ExitStack() as st:
            esems = {}
            for e in ENGS:
                n = (nsig[e] + self.MAXV - 1) // self.MAXV
                esems[e] = [st.enter_context(nc.semaphore(f"s_{e}_{i}")) for i in range(max(n, 1))]
            dsems = {}
            for e in ENGS:
                if self.ndma[e]:
                    for i in range(self.ND):
                        dsems[(e, i)] = st.enter_context(nc.semaphore(f"d_{e}_{i}"))
            observed = {e: {f: 0 for f in ENGS} for e in ENGS}
            dseen = {e: {} for e in ENGS}
            snaps = {}
            plan = {e: [] for e in ENGS}
            for op in self.ops:
                E = op.eng
                obs = observed[E]
                waits = []
                if op.prewait is not None:
                    ds, dv = op.prewait
                    if dseen[E].get(ds, 0) < dv:
                        dseen[E][ds] = dv
                        waits.append(("d", ds, dv))
                need = {}
                for did in sorted(op.deps):
                    d = self.ops[did]
                    if d.dma:
                        if dseen[E].get(d.dsem, 0) < d.dval:
                            dseen[E][d.dsem] = d.dval
                            waits.append(("d", d.dsem, d.dval))
                    else:
                        n = abs(d.ord)
                        if n > need.get(d.eng, 0):
                            need[d.eng] = n
                for F, n in need.items():
                    if obs[F] >= n:
                        continue
                    waits.append(("e", F, n))
                    obs[F] = n
                    sn = snaps.get((F, n))
                    if sn is not None:
                        for G, v in sn.items():
                            if v > obs[G]:
                                obs[G] = v
                if (not op.dma) and op.signal:
                    s = dict(obs)
                    s[E] = max(s[E], op.ord - 1)
                    snaps[(E, op.ord)] = s
                plan[E].append((op, waits))
            self.n_waits = sum(len(w) for e in ENGS for (_, w) in plan[e])
            MAXV = self.MAXV

            def semval(F, n):
                return esems[F][(n - 1) // MAXV], ((n - 1) % MAXV) + 1

            def run(e, eng):
                for op, waits in plan[e]:
                    for w in waits:
                        if w[0] == "d":
                            eng.wait_ge(dsems[w[1]], w[2])
                        else:
                            sem, v = semval(w[1], w[2])
                            eng.wait_ge(sem, v)
                    ins = op.fn(eng)
                    if op.dma:
                        ins.then_inc(dsems[op.dsem], 16)
                    elif op.signal:
                        sem, _ = semval(e, op.ord)
                        ins.then_inc(sem, 1)
                if e == "sp":
                    for q in ENGS:
                        k = self.ndma[q]
                        for slot in range(min(k, self.ND)):
                            cnt = (k - 1 - slot) // self.ND + 1
                            eng.wait_ge(dsems[(q, slot)], 16 * cnt)

            with nc.Block() as block:
                deco = {"pe": block.tensor, "dve": block.vector, "act": block.scalar,
                        "pool": block.gpsimd, "sp": block.sync}
                for e in ENGS:
                    if self.eops[e] or e == "sp":
                        deco[e](lambda eng, e=e: run(e, eng))

D = 1024
CT = 256
EPS = 1e-6
DFF = 2816
DFE = 3584
NEXP = 8


class Arena:
    def __init__(self, ap_f32):
        self.ap = ap_f32
        self.nw = ap_f32.shape[1]
        self.off = 0

    def reset(self, off=0):
        self.off = off

    def alloc(self, shape, dt=F32, parts=128):
        n = 1
        for d in shape[1:]:
            n *= d
        nbytes = n * _esz(dt)
        nwords = (nbytes + 3) // 4
        nwords = (nwords + 7) // 8 * 8
        assert self.off + nwords <= self.nw, ("arena overflow", self.off, nwords, self.nw)
        v = self.ap[0:parts, self.off:self.off + nwords]
        self.off += nwords
        if dt != F32:
            v = v.bitcast(dt)
        v = v[:, 0:n]
        if len(shape) == 2:
            return v
        names = " ".join(f"d{i}" for i in range(1, len(shape)))
        kw = {f"d{i}": shape[i] for i in range(1, len(shape))}
        return v.rearrange(f"p ({names}) -> p {names}", **kw)


def bc_rows(ap2d_row, parts=128):
    return ap2d_row.to_broadcast([parts, ap2d_row.shape[1]])


def dram_ap(ap, offset_elems, pattern):
    return bass.AP(ap.tensor, offset_elems, pattern)


class MK:
    def __init__(self, T, L=2, dbg=(), stages=None, half=False):
        self.half = half
        self.T = T
        self.L = L
        self.S_ = T + CT
        self.NT = self.S_ // 128
        self.NLT = T // 128
        self.dbg = set(dbg)
        self.stages = stages
        self.nc = bass.Bass("TRN2", target_bir_lowering=False)
        self.st = contextlib.ExitStack()
        self.S = Sched(self.nc)
        self.outs = []

    def din(self, name, shape, dt=F32):
        return self.nc.dram_tensor(name, list(shape), dt, kind="ExternalInput").ap()

    def dsc(self, name, shape, dt=F32):
        if name in self.dbg:
            self.outs.append(name)
            return self.nc.dram_tensor(name, list(shape), dt, kind="ExternalOutput").ap()
        return self.nc.dram_tensor(name, list(shape), dt).ap()

    def sb(self, name, shape, dt=F32):
        return self.st.enter_context(self.nc.sbuf_tensor(name, list(shape), dt))

    def declare(self):
        T, L, S_ = self.T, self.L, self.S_
        i = {}
        i["x"] = self.din("x", [T, D])
        i["ctx"] = self.din("ctx", [CT, D])
        i["cvec"] = self.din("cvec", [2, D])
        i["ada_w"] = self.din("ada_w", [L, D, 6 * D])
        i["ada_b"] = self.din("ada_b", [L, 6 * D])
        i["norm_mix"] = self.din("norm_mix", [L, D])
        i["norm_ffn"] = self.din("norm_ffn", [L, D])
        i["w_in"] = self.din("w_in", [L, D, 2416])
        i["w_out"] = self.din("w_out", [L, D, D])
        i["ml_gate_b"] = self.din("ml_gate_b", [L, 16])
        i["ml_out_norm"] = self.din("ml_out_norm", [L, 256])
        i["mla_q_norm"] = self.din("mla_q_norm", [L, 192])
        i["mla_w_uq"] = self.din("mla_w_uq", [L, 192, 384])
        i["mla_kv_norm"] = self.din("mla_kv_norm", [L, 128])
        i["mla_w_ukv"] = self.din("mla_w_ukv", [L, 128, 512])
        i["mla_q_gain"] = self.din("mla_q_gain", [L, 96])
        i["mla_k_gain"] = self.din("mla_k_gain", [L, 96])
        i["sw_q_gain"] = self.din("sw_q_gain", [L, 64])
        i["sw_k_gain"] = self.din("sw_k_gain", [L, 64])
        i["sw_sink"] = self.din("sw_sink", [L, 4])
        i["lru_conv_w"] = self.din("lru_conv_w", [L, 5, 256])
        i["lru_conv_b"] = self.din("lru_conv_b", [L, 256])
        i["lru_wa"] = self.din("lru_wa", [L, 2, 4, 64, 64])
        i["lru_ba"] = self.din("lru_ba", [L, 2, 256])
        i["lru_wx"] = self.din("lru_wx", [L, 2, 4, 64, 64])
        i["lru_bx"] = self.din("lru_bx", [L, 2, 256])
        i["lru_lam"] = self.din("lru_lam", [L, 2, 256])
        i["ffn_w13"] = self.din("ffn_w13", [1, D, 2 * DFF])
        i["ffn_w2"] = self.din("ffn_w2", [1, DFF, D])
        i["moe_router"] = self.din("moe_router", [1, D, NEXP])
        i["moe_router_b"] = self.din("moe_router_b", [1, NEXP])
        i["moe_w13"] = self.din("moe_w13", [1, NEXP, D, 2 * DFE])
        i["moe_w2"] = self.din("moe_w2", [1, NEXP, DFE, D])
        i["cst"] = self.din("cst", [128, 768])
        i["ropeB"] = self.din("ropeB", [T, 2, 32])
        i["ropeC"] = self.din("ropeC", [T, 2, 64])
        self.i = i
        self.To = T // 2 if self.half else T
        self.out = self.nc.dram_tensor("out", [self.To, D], F32, kind="ExternalOutput").ap()
        d = {}
        d["modd"] = self.dsc("modd", [L, 2, 6 * D])
        d["X1"] = self.dsc("X1", [S_, D])
        d["A_qT"] = self.dsc("A_qT", [64, 4, S_], BF16)
        d["A_kT"] = self.dsc("A_kT", [64, 4, S_], BF16)
        d["A_k"] = self.dsc("A_k", [S_, 256], BF16)
        d["A_v"] = self.dsc("A_v", [S_, 4, 65], BF16)
        d["A_so"] = self.dsc("A_so", [S_, 256], BF16)
        d["A_g"] = self.dsc("A_g", [S_, 16])
        d["A_hf"] = self.dsc("A_hf", [S_, 256])
        d["B_QT"] = self.dsc("B_QT", [96, 4, S_], BF16)
        d["B_KT"] = self.dsc("B_KT", [96, 4, S_], BF16)
        d["B_V"] = self.dsc("B_V", [S_, 4, 65], BF16)
        d["C_QT"] = self.dsc("C_QT", [64, 4, S_], BF16)
        d["C_KT"] = self.dsc("C_KT", [64, 2, S_], BF16)
        d["C_V"] = self.dsc("C_V", [S_, 2, 65], BF16)
        d["D_u"] = self.dsc("D_u", [128, 2, S_])
        d["D_g"] = self.dsc("D_g", [128, 2, S_])
        d["D_hf"] = self.dsc("D_hf", [128, 2, S_])
        d["Y"] = self.dsc("Y", [S_, 768], BF16)
        d["YTD"] = self.dsc("YTD", [128, 2, S_], BF16)
        d["W13d"] = self.dsc("W13d", [11, 128, 8, 2, 256], BF16)
        d["W2d"] = self.dsc("W2d", [DFF, D], BF16)
        d["W13e"] = self.dsc("W13e", [NEXP, 7, 128, 8, 2, 512], BF16)
        d["W2e"] = self.dsc("W2e", [NEXP, DFE, D], BF16)
        self.d = d
        self.cst = self.sb("cst_sb", [128, 768])
        self.cstb = self.sb("cstb_sb", [128, 768], BF16)
        self.ident = self.cst[:, 0:128]
        self.triU = self.cst[:, 128:256]
        self.triL = self.cst[:, 256:384]
        self.triUs = self.cst[:, 384:512]
        self.triLs = self.cst[:, 512:640]
        self.ones = self.cst[:, 640:768]
        self.identb = self.cstb[:, 0:128]
        self.triUb = self.cstb[:, 128:256]
        self.triLb = self.cstb[:, 256:384]
        self.vec = self.sb("vec_sb", [128, 256])
        self.bc = self.sb("bc_sb", [128, 4, D])
        self.smallbc = self.sb("smallbc_sb", [128, 2048])
        self.PS = [self.st.enter_context(self.nc.psum_tensor(f"ps{k}", [128, 512], F32)) for k in range(8)]
        rem = self.nc.sbuf_bytes_remaining
        nw = (rem - 4096) // 4
        nw = nw // 8 * 8
        self.arena_t = self.sb("arena", [128, nw])
        self.ar = Arena(self.arena_t[:, :])
        self.arena_words = nw

    def PSb(self, k):
        return self.PS[k][:, :].bitcast(BF16)

    def prep(self):
        S, i, d = self.S, self.i, self.d
        L = self.L
        S.dma(self.cst[:, :], i["cst"][:, :])
        S.copy(self.cstb[:, :], self.cst[:, :])
        ar = self.ar
        ar.reset()
        cT = ar.alloc([128, 8, 2])
        for r in range(2):
            S.dma(cT[:, :, r], i["cvec"][r:r + 1, :].rearrange("o (c p) -> p (o c)", p=128), allow_slow_non_contiguous=True)
        cS = ar.alloc([128, 8, 2])
        S.act(cS, cT, AF.Silu)
        awr = [ar.alloc([128, 8, 512]) for _ in range(2)]
        abr = [ar.alloc([2, 512], parts=2) for _ in range(2)]
        mo = [ar.alloc([2, 512], parts=2) for _ in range(2)]
        k = 0
        for l in range(L):
            for ng in range(12):
                aw = awr[k % 2]
                ab = abr[k % 2]
                S.dma(aw, i["ada_w"][l, :, ng * 512:(ng + 1) * 512].rearrange("(c p) n -> p c n", p=128))
                S.dma(ab, i["ada_b"][l:l + 1, ng * 512:(ng + 1) * 512].to_broadcast([2, 512]))
                ps = self.PS[k % 2][0:2, :]
                for c in range(8):
                    S.mm(ps, cS[:, c, :], aw[:, c, :], start=(c == 0), stop=(c == 7))
                m = mo[k % 2]
                S.tt(m, ps, ab, ALU.add)
                S.dma(d["modd"][l, :, ng * 512:(ng + 1) * 512], m)
                k += 1
        w13 = i["ffn_w13"]
        for pc in range(11):
            for kc in range(8):
                src = dram_ap(w13, kc * 128 * 2 * DFF + pc * 256, [[2 * DFF, 128], [DFF, 2], [1, 256]])
                S.dma(d["W13d"][pc, :, kc, :, :], src, q="pool")
        for r in range(0, DFF, 704):
            S.dma(d["W2d"][r:r + 704, :], i["ffn_w2"][0, r:r + 704, :], q="pool")

    def prep_moe(self):
        S, i, d = self.S, self.i, self.d
        w13 = i["moe_w13"]
        for e in range(NEXP):
            for pc in range(7):
                for kc in range(8):
                    src = dram_ap(w13, e * D * 2 * DFE + kc * 128 * 2 * DFE + pc * 512,
                                  [[2 * DFE, 128], [DFE, 2], [1, 512]])
                    S.dma(d["W13e"][e, pc, :, kc, :, :], src, q="pool")
            for r in range(0, DFE, 896):
                S.dma(d["W2e"][e, r:r + 896, :], i["moe_w2"][0, e, r:r + 896, :], q="pool")

    def layer_setup(self, l):
        S, i, d = self.S, self.i, self.d
        vec = self.vec
        def col(src_row_ap, c0):
            S.dma(vec[:, c0:c0 + 8], src_row_ap.rearrange("o (c p) -> p (o c)", p=128), allow_slow_non_contiguous=True)
        col(i["norm_mix"][l:l + 1, :], 0)
        col(i["norm_ffn"][l:l + 1, :], 8)
        for r in range(2):
            b0 = 16 + r * 48
            md = d["modd"]
            col(md[l, r:r + 1, 0:D], b0)
            col(md[l, r:r + 1, D:2 * D], b0 + 8)
            col(md[l, r:r + 1, 3 * D:4 * D], b0 + 24)
            col(md[l, r:r + 1, 4 * D:5 * D], b0 + 32)
            S.stt(vec[:, b0 + 16:b0 + 24], vec[:, b0 + 8:b0 + 16], 1.0, vec[:, 0:8], ALU.add, ALU.mult)
            S.stt(vec[:, b0 + 40:b0 + 48], vec[:, b0 + 32:b0 + 40], 1.0, vec[:, 8:16], ALU.add, ALU.mult)
            S.dma(self.bc[:, r, :], md[l, r:r + 1, 2 * D:3 * D].to_broadcast([128, D]))
            S.dma(self.bc[:, 2 + r, :], md[l, r:r + 1, 5 * D:6 * D].to_broadcast([128, D]))
        sb_ = self.smallbc
        o = 0
        def bcl(name, src_ap_1d_row, n, reps=1):
            nonlocal o
            dst = sb_[:, o:o + n * reps]
            t = src_ap_1d_row
            if reps == 1:
                S.dma(dst, t.to_broadcast([128, n]))
            else:
                srcap = dram_ap(t, int(t.offset), [[0, 128], [0, reps], [1, n]])
                S.dma(dst.rearrange("p (r n) -> p r n", r=reps), srcap)
            setattr(self, name, dst)
            o += n * reps
        bcl("gateb_bc", i["ml_gate_b"][l:l + 1, :], 16)
        bcl("mlon_bc", i["ml_out_norm"][l:l + 1, :], 256)
        bcl("qnorm_bc", i["mla_q_norm"][l:l + 1, :], 192)
        bcl("kvnorm_bc", i["mla_kv_norm"][l:l + 1, :], 128)
        bcl("qgn_bc", i["mla_q_gain"][l:l + 1, 0:64], 64, 4)
        bcl("qgr_bc", i["mla_q_gain"][l:l + 1, 64:96], 32, 4)
        bcl("kgn_bc", i["mla_k_gain"][l:l + 1, 0:64], 64, 4)
        bcl("kgr_bc", i["mla_k_gain"][l:l + 1, 64:96], 32)
        bcl("swq_bc", i["sw_q_gain"][l:l + 1, :], 64, 4)
        bcl("swk_bc", i["sw_k_gain"][l:l + 1, :], 64, 2)
        bcl("sink_bc", i["sw_sink"][l:l + 1, :], 4)
        assert o <= 2048
        self.swqk_bc = sb_[:, (o - 4 - 128 - 256):(o - 4)]
        S.act(self.sink_bc, self.sink_bc, AF.Exp)

    def norm_mod_T(self, xt, gm, sh, hT_out, tmp, psb, f32_out=None, psf=None):
        S = self.S
        junk, ssq, xn = tmp["junk"], tmp["ssq"], tmp["xn"]
        S.act(junk, xt, AF.Square, accum_out=ssq)
        S.ts(ssq, ssq, 1.0 / D, EPS, ALU.mult, ALU.add)
        S.act(ssq, ssq, AF.Sqrt)
        S.recip(ssq, ssq)
        if f32_out is None:
            S.ts(xn, xt, ssq, None, ALU.mult)
            for c in range(8):
                S.transpose(psb[:, c * 128:(c + 1) * 128], xn[:, c * 128:(c + 1) * 128], self.identb)
            for c in range(8):
                S.act(hT_out[:, c, :], psb[:, c * 128:(c + 1) * 128], AF.Identity,
                      bias=sh[:, c:c + 1], scale=gm[:, c:c + 1])
        else:
            xn32 = tmp["xn32"]
            S.ts(xn32, xt, ssq, None, ALU.mult)
            for c in range(8):
                pf = psf[c // 4][:, (c % 4) * 128:(c % 4 + 1) * 128]
                S.transpose(pf, xn32[:, c * 128:(c + 1) * 128], self.ident)
            for c in range(8):
                pf = psf[c // 4][:, (c % 4) * 128:(c % 4 + 1) * 128]
                S.act(f32_out[:, c, :], pf, AF.Identity, bias=sh[:, c:c + 1], scale=gm[:, c:c + 1])
                S.copy(hT_out[:, c, :], f32_out[:, c, :], eng="pool")

    def rstd(self, ss, n_inv):
        S = self.S
        S.ts(ss, ss, n_inv, EPS, ALU.mult, ALU.add)
        S.act(ss, ss, AF.Sqrt)
        S.recip(ss, ss)

    def rope(self, out, x, tab, n, tmp1, tmp2):
        S = self.S
        e = n // 4
        S.tt(tmp1, x, tab[:, 0, :], ALU.mult)
        xs = x.rearrange("p (g h e) -> p g h e", h=2, e=e)[:, :, ::-1, :]
        S.tt(tmp2.rearrange("p (g h e) -> p g h e", h=2, e=e), xs,
             tab[:, 1, :].rearrange("p (g h e) -> p g h e", h=2, e=e), ALU.mult)
        S.tt(out, tmp1, tmp2, ALU.add)

    def P1(self, l):
        S, i, d = self.S, self.i, self.d
        T, NT = self.T, self.NT
        ar = self.ar
        ar.reset()
        ctx_out = l < self.L - 1
        win = ar.alloc([128, 8, 2416], BF16)
        stg = ar.alloc([128, 8, 2416])
        for c in range(8):
            S.dma(stg[:, c, :], i["w_in"][l, c * 128:(c + 1) * 128, :])
            S.copy(win[:, c, :], stg[:, c, :], eng="pool")
        wuq = ar.alloc([128, 2, 384], BF16)
        wuqs = ar.alloc([128, 2, 384])
        S.dma(wuqs[:, 0, :], i["mla_w_uq"][l, 0:128, :])
        S.dma(wuqs[0:64, 1, :], i["mla_w_uq"][l, 128:192, :])
        S.copy(wuq[:, 0, :], wuqs[:, 0, :], eng="pool")
        S.copy(wuq[0:64, 1, :], wuqs[0:64, 1, :], eng="pool")
        wukv = ar.alloc([128, 512], BF16)
        wukvs = ar.alloc([128, 512])
        S.dma(wukvs, i["mla_w_ukv"][l])
        S.copy(wukv, wukvs, eng="pool")
        xr = [ar.alloc([128, D]) for _ in range(2)]
        tmp = {"junk": ar.alloc([128, D]), "ssq": ar.alloc([128, 1]), "xn": ar.alloc([128, D], BF16)}
        hT = ar.alloc([128, 8, 128], BF16)
        qkT = ar.alloc([128, 4, 128], BF16)
        ktm = ar.alloc([128, 256], BF16)
        vA = ar.alloc([128, 4, 65], BF16)
        S.memset(vA[:, :, 64:65], 1.0)
        soA = ar.alloc([128, 256], BF16)
        g16 = ar.alloc([128, 16])
        gtmp = ar.alloc([128, 8])
        zb = ar.alloc([128, 352])
        ss3 = ar.alloc([128, 4])
        cqn = ar.alloc([128, 192], BF16)
        ckvn = ar.alloc([128, 128], BF16)
        krn = ar.alloc([128, 32])
        krr = ar.alloc([128, 32])
        rt1 = ar.alloc([128, 384])
        rt2 = ar.alloc([128, 384])
        cqT = ar.alloc([128, 2, 128], BF16)
        ckvT = ar.alloc([128, 128], BF16)
        sq = ar.alloc([128, 512])
        ss8 = ar.alloc([128, 8])
        qn = ar.alloc([128, 384])
        Qh = ar.alloc([128, 4, 96], BF16)
        Kh = ar.alloc([128, 4, 96], BF16)
        vB = ar.alloc([128, 4, 65], BF16)
        S.memset(vB[:, :, 64:65], 1.0)
        QKT = ar.alloc([96, 8, 128], BF16, parts=96)
        tabB = ar.alloc([128, 2, 128])
        tabB1 = ar.alloc([128, 2, 32])
        tabC = ar.alloc([128, 2, 384])
        zc = ar.alloc([128, 512])
        ss6 = ar.alloc([128, 8])
        qkc = ar.alloc([128, 384])
        qkcb = ar.alloc([128, 6, 64], BF16)
        vC = ar.alloc([128, 2, 65], BF16)
        S.memset(vC[:, :, 64:65], 1.0)
        qkcT = ar.alloc([64, 6, 128], BF16, parts=64)
        zd = ar.alloc([128, 4, 128])
        PS = self.PS
        for ti in range(NT):
            role = 1 if ti < 2 else 0
            s0 = ti * 128
            lat = ti >= 2
            t0 = (ti - 2) * 128
            halfL = self.half and (l == self.L - 1)
            need_q = (lat and (not halfL or t0 < T // 2)) or ctx_out
            if l == 0:
                src = i["ctx"][s0:s0 + 128, :] if not lat else i["x"][t0:t0 + 128, :]
            else:
                src = d["X1"][s0:s0 + 128, :]
            xt = xr[ti % 2]
            S.dma(xt, src)
            b0 = 16 + role * 48
            gm, sh = self.vec[:, b0 + 16:b0 + 24], self.vec[:, b0:b0 + 8]
            self.norm_mod_T(xt, gm, sh, hT, tmp, self.PSb(6))
            if lat:
                for a in range(2):
                    S.dma(tabB[:, a, :].rearrange("p (h n) -> p h n", h=4),
                          dram_ap(i["ropeB"], t0 * 64 + a * 32, [[64, 128], [0, 4], [1, 32]]))
                    S.dma(tabC[:, a, :].rearrange("p (h n) -> p h n", h=6),
                          dram_ap(i["ropeC"], t0 * 128 + a * 64, [[128, 128], [0, 6], [1, 64]]))
                S.dma(tabB1, i["ropeB"][t0:t0 + 128, :, :])
            groups = [(0, 0, 512), (1, 512, 512), (2, 1024, 368), (3, 1392, 512)]
            for (pk, off, w) in groups:
                for c in range(8):
                    S.mm(PS[pk][:, 0:w], hT[:, c, :], win[:, c, off:off + w], start=(c == 0), stop=(c == 7), signal=(c == 7))
            for j in range(4):
                for c in range(8):
                    S.mm(PS[4][:, j * 128:(j + 1) * 128], win[:, c, j * 128:(j + 1) * 128], hT[:, c, :],
                         start=(c == 0), stop=(c == 7), signal=(c == 7))
            for j in range(4):
                for c in range(8):
                    S.mm(PS[5][:, j * 128:(j + 1) * 128], win[:, c, 1904 + j * 128:1904 + (j + 1) * 128], hT[:, c, :],
                         start=(c == 0), stop=(c == 7), signal=(c == 7))
            S.copy(qkT[:, 0:2, :], PS[4][:, 0:256].rearrange("p (a t) -> p a t", a=2), eng="act")
            S.act(qkT[:, 2:4, :], PS[4][:, 256:512].rearrange("p (a t) -> p a t", a=2), AF.Copy, scale=0.125)
            for hp in range(2):
                S.dma(d["A_qT"][:, hp::2, s0:s0 + 128], qkT[hp * 64:(hp + 1) * 64, 0:2, :])
                S.dma(d["A_kT"][:, hp::2, s0:s0 + 128], qkT[hp * 64:(hp + 1) * 64, 2:4, :])
            S.act(ktm, PS[0][:, 256:512], AF.Copy, scale=0.125)
            S.dma(d["A_k"][s0:s0 + 128, :], ktm)
            S.copy(vA[:, :, 0:64], PS[1][:, 0:256].rearrange("p (h e) -> p h e", h=4))
            S.dma(d["A_v"][s0:s0 + 128, :, :], vA)
            S.act(soA, PS[1][:, 256:512], AF.Sigmoid)
            S.dma(d["A_so"][s0:s0 + 128, :], soA)
            S.tt(g16, PS[2][:, 0:16], self.gateb_bc, ALU.add)
            gv = g16.rearrange("p (g k) -> p g k", k=4)[:, 1::2, :]
            gt = gtmp.rearrange("p (g k) -> p g k", k=4)
            S.act(gt, gv, AF.Exp, scale=-1.0)
            S.act(gt, gt, AF.Ln, bias=1.0)
            S.ts(gv, gt, -1.0, None, ALU.mult)
            S.dma(d["A_g"][s0:s0 + 128, :], g16)
            S.copy(zd, PS[5][:, :].rearrange("p (a t) -> p a t", a=4))
            S.dma(d["D_u"][:, :, s0:s0 + 128], zd[:, 0:2, :])
            S.dma(d["D_g"][:, :, s0:s0 + 128], zd[:, 2:4, :])
            S.copy(zc, PS[3][:, :], eng="act")
            S.tt(sq[:, 0:384], zc[:, 0:384], zc[:, 0:384], ALU.mult, eng="pool")
            S.reduce(ss6[:, 0:6], sq[:, 0:384].rearrange("p (h e) -> p h e", e=64), ALU.add)
            self.rstd(ss6[:, 0:6], 1.0 / 64)
            S.tt(qkc.rearrange("p (h e) -> p h e", e=64), zc[:, 0:384].rearrange("p (h e) -> p h e", e=64),
                 ss6[:, 0:6].unsqueeze(2).to_broadcast([128, 6, 64]), ALU.mult)
            if lat:
                S.tt(qkc, qkc, self.swqk_bc, ALU.mult)
                self.rope(qkcb.rearrange("p h e -> p (h e)"), qkc, tabC, 64, rt1, rt2)
            else:
                S.tt(qkcb.rearrange("p h e -> p (h e)"), qkc, self.swqk_bc, ALU.mult)
            pb = self.PSb(7)
            for h in range(6):
                S.transpose(pb[0:64, h * 128:(h + 1) * 128], qkcb[:, h, :], self.identb)
            S.copy(qkcT, pb[0:64, 0:768].rearrange("p (h t) -> p h t", h=6), eng="act")
            if need_q:
                S.dma(d["C_QT"][:, :, s0:s0 + 128], qkcT[:, 0:4, :])
            S.dma(d["C_KT"][:, :, s0:s0 + 128], qkcT[:, 4:6, :])
            S.copy(vC[:, :, 0:64], zc[:, 384:512].rearrange("p (h e) -> p h e", h=2))
            S.dma(d["C_V"][s0:s0 + 128, :, :], vC)
            S.copy(zb, PS[2][:, 16:368], eng="act")
            S.tt(sq[:, 0:352], zb, zb, ALU.mult, eng="pool")
            S.reduce(ss3[:, 0:1], sq[:, 0:192], ALU.add)
            S.reduce(ss3[:, 1:2], sq[:, 192:320], ALU.add)
            S.reduce(ss3[:, 2:3], sq[:, 320:352], ALU.add)
            S.ts(ss3[:, 0:1], ss3[:, 0:1], 1.0 / 192, None, ALU.mult)
            S.ts(ss3[:, 1:2], ss3[:, 1:2], 1.0 / 128, None, ALU.mult)
            S.ts(ss3[:, 2:3], ss3[:, 2:3], 1.0 / 32, None, ALU.mult)
            self.rstd(ss3[:, 0:3], 1.0)
            S.stt(ckvn, zb[:, 192:320], ss3[:, 1:2], self.kvnorm_bc, ALU.mult, ALU.mult)
            S.stt(krn, zb[:, 320:352], ss3[:, 2:3], self.kgr_bc, ALU.mult, ALU.mult)
            if lat:
                self.rope(krr, krn, tabB1, 32, rt1[:, 0:32], rt2[:, 0:32])
                krsrc = krr
            else:
                krsrc = krn
            pb6 = self.PSb(6)
            S.transpose(pb6[:, 256:384], ckvn, self.identb)
            S.copy(ckvT, pb6[:, 256:384], eng="act")
            S.mm(PS[5][:, :], ckvT, wukv)
            S.act(sq[:, 0:256], PS[5][:, 0:256], AF.Square)
            S.reduce(ss8[:, 0:4], sq[:, 0:256].rearrange("p (h e) -> p h e", e=64), ALU.add)
            self.rstd(ss8[:, 0:4], 1.0 / 64)
            S.tt(qn[:, 0:256].rearrange("p (h e) -> p h e", e=64), PS[5][:, 0:256].rearrange("p (h e) -> p h e", e=64),
                 ss8[:, 0:4].unsqueeze(2).to_broadcast([128, 4, 64]), ALU.mult)
            S.tt(Kh[:, :, 0:64], qn[:, 0:256].rearrange("p (h e) -> p h e", e=64),
                 self.kgn_bc.rearrange("p (h e) -> p h e", e=64), ALU.mult)
            S.copy(Kh[:, :, 64:96], krsrc.unsqueeze(1).to_broadcast([128, 4, 32]), eng="pool")
            S.copy(vB[:, :, 0:64], PS[5][:, 256:512].rearrange("p (h e) -> p h e", h=4), eng="act")
            S.dma(d["B_V"][s0:s0 + 128, :, :], vB)
            for h in range(4):
                S.transpose(pb6[0:96, 512 + h * 128:512 + (h + 1) * 128], Kh[:, h, :], self.identb)
            if need_q:
                S.stt(cqn, zb[:, 0:192], ss3[:, 0:1], self.qnorm_bc, ALU.mult, ALU.mult)
                S.transpose(pb6[:, 0:128], cqn[:, 0:128], self.identb)
                S.transpose(pb6[0:64, 128:256], cqn[:, 128:192], self.identb)
                S.copy(cqT[:, 0, :], pb6[:, 0:128], eng="act")
                S.copy(cqT[0:64, 1, :], pb6[0:64, 128:256], eng="act")
                S.mm(PS[4][:, 0:384], cqT[:, 0, :], wuq[:, 0, :], start=True, stop=False)
                S.mm(PS[4][:, 0:384], cqT[0:64, 1, :], wuq[0:64, 1, :], start=False, stop=True)
                S.act(sq[:, 0:384], PS[4][:, 0:384], AF.Square)
                S.reduce(ss8[:, 0:4], sq[:, 0:256].rearrange("p (h e) -> p h e", e=64), ALU.add)
                S.reduce(ss8[:, 4:8], sq[:, 256:384].rearrange("p (h e) -> p h e", e=32), ALU.add)
                S.ts(ss8[:, 0:4], ss8[:, 0:4], 1.0 / 64, None, ALU.mult)
                S.ts(ss8[:, 4:8], ss8[:, 4:8], 1.0 / 32, None, ALU.mult)
                self.rstd(ss8[:, 0:8], 1.0)
                S.tt(qn[:, 0:256].rearrange("p (h e) -> p h e", e=64), PS[4][:, 0:256].rearrange("p (h e) -> p h e", e=64),
                     ss8[:, 0:4].unsqueeze(2).to_broadcast([128, 4, 64]), ALU.mult)
                S.tt(Qh[:, :, 0:64], qn[:, 0:256].rearrange("p (h e) -> p h e", e=64),
                     self.qgn_bc.rearrange("p (h e) -> p h e", e=64), ALU.mult)
                S.tt(qn[:, 256:384].rearrange("p (h e) -> p h e", e=32), PS[4][:, 256:384].rearrange("p (h e) -> p h e", e=32),
                     ss8[:, 4:8].unsqueeze(2).to_broadcast([128, 4, 32]), ALU.mult)
                S.tt(qn[:, 256:384], qn[:, 256:384], self.qgr_bc, ALU.mult)
                if lat:
                    self.rope(sq[:, 384:512], qn[:, 256:384], tabB, 32, rt1[:, 0:128], rt2[:, 0:128])
                    S.copy(Qh[:, :, 64:96], sq[:, 384:512].rearrange("p (h e) -> p h e", e=32), eng="pool")
                else:
                    S.copy(Qh[:, :, 64:96], qn[:, 256:384].rearrange("p (h e) -> p h e", e=32), eng="pool")
                pb7 = self.PSb(7)
                for h in range(4):
                    S.transpose(pb7[0:96, h * 128:(h + 1) * 128], Qh[:, h, :], self.identb)
                S.copy(QKT[:, 0:4, :], pb7[0:96, 0:512].rearrange("p (h t) -> p h t", h=4), eng="act")
                S.dma(d["B_QT"][:, :, s0:s0 + 128], QKT[:, 0:4, :])
            S.copy(QKT[:, 4:8, :], pb6[0:96, 512:1024].rearrange("p (h t) -> p h t", h=4), eng="act")
            S.dma(d["B_KT"][:, :, s0:s0 + 128], QKT[:, 4:8, :])

    def mix_D(self, l):
        S, i, d = self.S, self.i, self.d
        T, S_ = self.T, self.S_
        ctx_out = l < self.L - 1
        ar = self.ar
        ar.reset()
        halfL = self.half and (l == self.L - 1)
        cw = ar.alloc([128, 2, 5])
        for kk in range(5):
            S.dma(cw[:, :, kk], i["lru_conv_w"][l, kk:kk + 1, :].rearrange("o (c p) -> p (o c)", p=128),
                  allow_slow_non_contiguous=True)
        cb = ar.alloc([128, 2])
        S.dma(cb, i["lru_conv_b"][l:l + 1, :].rearrange("o (c p) -> p (o c)", p=128), allow_slow_non_contiguous=True)
        def dirvec(name):
            t = ar.alloc([128, 2, 2])
            for dd in range(2):
                S.dma(t[:, dd, :], i[name][l, dd:dd + 1, :].rearrange("o (c p) -> p (o c)", p=128),
                      allow_slow_non_contiguous=True)
            return t
        ba, bx, lam = dirvec("lru_ba"), dirvec("lru_bx"), dirvec("lru_lam")
        nsc = ar.alloc([128, 2, 2])
        nsc2 = ar.alloc([128, 2, 2])
        S.act(nsc, lam, AF.Exp, scale=-1.0)
        S.act(nsc, nsc, AF.Ln, bias=1.0)
        S.ts(nsc2, nsc, -16.0, None, ALU.mult)
        S.ts(nsc, nsc, -8.0, None, ALU.mult)
        Wbd = ar.alloc([128, 8, 128])
        S.memset(Wbd, 0.0)
        for dr in range(2):
            for gi, nm in enumerate(("lru_wa", "lru_wx")):
                for cc in range(2):
                    for j in range(2):
                        S.dma(Wbd[j * 64:(j + 1) * 64, dr * 4 + gi * 2 + cc, j * 64:(j + 1) * 64],
                              i[nm][l, dr, cc * 2 + j, :, :])
        hst = ar.alloc([128, 2, 2])
        S.memset(hst, 0.0)
        NB = 512 if T >= 1024 else 256
        def mk():
            return ar.alloc([128, 2, NB])
        uh = [ar.alloc([128, 2, NB + 4]) for _ in range(2)]
        uc, r_, a_, a2_, i_, bb, hh, gg, hf = mk(), mk(), mk(), mk(), mk(), mk(), mk(), mk(), mk()
        yb = ar.alloc([128, 2, NB], BF16)
        blocks = [(0, CT, 0, CT)]
        for b0 in range(CT, S_, NB):
            blocks.append((CT, S_, b0, min(NB, S_ - b0)))
        Tq = T // 2 if halfL else T
        order_f = [bi for bi in range(len(blocks)) if blocks[bi][2] < CT + Tq]
        order_b = [0] + list(range(len(blocks) - 1, 0, -1))
        k = 0
        for dr, order in ((0, order_f), (1, order_b)):
            for bi in order:
                seg0, seg1, t0, n = blocks[bi]
                want_out = t0 < CT + Tq
                u = uh[k % 2]
                k += 1
                lo = max(seg0, t0 - 2)
                hi = min(seg1, t0 + n + 2)
                if lo > t0 - 2:
                    S.memset(u[:, :, 0:(lo - (t0 - 2))], 0.0)
                if hi < t0 + n + 2:
                    S.memset(u[:, :, (hi - (t0 - 2)):n + 4], 0.0)
                S.dma(u[:, :, (lo - (t0 - 2)):(hi - (t0 - 2))], d["D_u"][:, :, lo:hi])
                for cc in range(2):
                    S.ts(uc[:, cc, 0:n], u[:, cc, 0:n], cw[:, cc, 0:1], cb[:, cc:cc + 1], ALU.mult, ALU.add)
                    for kk in range(1, 5):
                        S.stt(uc[:, cc, 0:n], u[:, cc, kk:kk + n], cw[:, cc, kk:kk + 1], uc[:, cc, 0:n], ALU.mult, ALU.add)
                for cc in range(2):
                    S.mm(self.PS[cc][:, 0:n], Wbd[:, dr * 4 + cc, :], uc[:, cc, 0:n])
                    S.mm(self.PS[2 + cc][:, 0:n], Wbd[:, dr * 4 + 2 + cc, :], uc[:, cc, 0:n])
                for cc in range(2):
                    S.act(r_[:, cc, 0:n], self.PS[cc][:, 0:n], AF.Sigmoid, bias=ba[:, dr, cc:cc + 1])
                    S.act(i_[:, cc, 0:n], self.PS[2 + cc][:, 0:n], AF.Sigmoid, bias=bx[:, dr, cc:cc + 1])
                for cc in range(2):
                    S.act(a_[:, cc, 0:n], r_[:, cc, 0:n], AF.Exp, scale=nsc[:, dr, cc:cc + 1])
                    S.act(a2_[:, cc, 0:n], r_[:, cc, 0:n], AF.Exp, scale=nsc2[:, dr, cc:cc + 1])
                S.act(a2_[:, :, 0:n], a2_[:, :, 0:n], AF.Sqrt, bias=1.0, scale=-1.0)
                S.tt(bb[:, :, 0:n], i_[:, :, 0:n], uc[:, :, 0:n], ALU.mult, eng="pool")
                S.tt(bb[:, :, 0:n], bb[:, :, 0:n], a2_[:, :, 0:n], ALU.mult, eng="pool")
                for cc in range(2):
                    if dr == 0:
                        S.scan(hh[:, cc, 0:n], a_[:, cc, 0:n], bb[:, cc, 0:n], hst[:, dr, cc:cc + 1], ALU.mult, ALU.add)
                        S.copy(hst[:, dr, cc:cc + 1], hh[:, cc, n - 1:n])
                    else:
                        S.scan(hh[:, cc, 0:n][:, ::-1], a_[:, cc, 0:n][:, ::-1], bb[:, cc, 0:n][:, ::-1],
                               hst[:, dr, cc:cc + 1], ALU.mult, ALU.add)
                        S.copy(hst[:, dr, cc:cc + 1], hh[:, cc, 0:1])
                if dr == 0:
                    S.dma(d["D_hf"][:, :, t0:t0 + n], hh[:, :, 0:n])
                else:
                    if (bi == 0 and not ctx_out) or not want_out:
                        continue
                    S.dma(hf[:, :, 0:n], d["D_hf"][:, :, t0:t0 + n])
                    S.dma(gg[:, :, 0:n], d["D_g"][:, :, t0:t0 + n])
                    S.tt(hh[:, :, 0:n], hh[:, :, 0:n], hf[:, :, 0:n], ALU.add)
                    S.tt(hf[:, :, 0:n], gg[:, :, 0:n], gg[:, :, 0:n], ALU.mult, eng="pool")
                    S.ts(hf[:, :, 0:n], hf[:, :, 0:n], 0.044715, 1.0, ALU.mult, ALU.add)
                    S.tt(hf[:, :, 0:n], hf[:, :, 0:n], gg[:, :, 0:n], ALU.mult, eng="pool")
                    S.act(hf[:, :, 0:n], hf[:, :, 0:n], AF.Sigmoid, scale=1.5957691216057308)
                    S.tt(hf[:, :, 0:n], hf[:, :, 0:n], gg[:, :, 0:n], ALU.mult, eng="pool")
                    S.tt(yb[:, :, 0:n], hf[:, :, 0:n], hh[:, :, 0:n], ALU.mult)
                    S.dma(d["YTD"][:, :, t0:t0 + n], yb[:, :, 0:n])

    def mix_C(self, l):
        S, i, d = self.S, self.i, self.d
        T, S_, NT, NLT = self.T, self.S_, self.NT, self.NLT
        ctx_out = l < self.L - 1
        ar = self.ar
        ar.reset()
        KT = ar.alloc([64, 2, S_], BF16, parts=64)
        for c0 in range(0, S_, 2048):
            c1 = min(S_, c0 + 2048)
            S.dma(KT[:, :, c0:c1], d["C_KT"][:, :, c0:c1])
        V = ar.alloc([128, NT, 2, 65], BF16)
        for t0 in range(0, NT, 16):
            t1 = min(NT, t0 + 16)
            S.dma(V[:, t0:t1, :, :], d["C_V"][t0 * 128:t1 * 128, :, :].rearrange("(t p) h e -> p t h e", p=128))
        QTr = [ar.alloc([64, 4, 128], BF16, parts=64) for _ in range(2)]
        PTr = [ar.alloc([128, 5, 256], BF16) for _ in range(2)]
        den = ar.alloc([128, 4])
        Yt = [ar.alloc([128, 256], BF16) for _ in range(2)]
        halfL = self.half and (l == self.L - 1)
        qblocks = list(range(2, 2 + (NLT // 2 if halfL else NLT))) + ([0, 1] if ctx_out else [])
        PS = self.PS
        k = 0
        for qi, ti in enumerate(qblocks):
            s0 = ti * 128
            lat = ti >= 2
            QT = QTr[qi % 2]
            S.dma(QT, d["C_QT"][:, :, s0:s0 + 128])
            if lat:
                chunks = [(0, None), (1, None)]
                if ti - 1 >= 2:
                    chunks.append((ti - 1, "L"))
                chunks.append((ti, None))
                if ti + 1 < NT:
                    chunks.append((ti + 1, "U"))
            else:
                chunks = [(0, None), (1, None)]
            Y = Yt[qi % 2]
            pso = PS[6 + (qi % 2)]
            for kvh in range(2):
                PT = PTr[k % 2]
                pss = [PS[(k % 2) * 3 + j] for j in range(3)]
                k += 1
                for ci, (kt, msk) in enumerate(chunks):
                    S.mm(pss[ci // 2][:, (ci % 2) * 256:(ci % 2 + 1) * 256].rearrange("p (g t) -> p g t", g=2),
                         KT[:, kvh, kt * 128:(kt + 1) * 128], QT[:, kvh * 2:kvh * 2 + 2, :])
                nch = len(chunks)
                for b in range((nch + 1) // 2):
                    w = min(2, nch - 2 * b)
                    S.act(PT[:, 2 * b:2 * b + w, :], pss[b][:, 0:w * 256].rearrange("p (c n) -> p c n", c=w),
                          AF.Exp, scale=0.125)
                for ci, (kt, msk) in enumerate(chunks):
                    if msk is not None:
                        mt = self.triLb if msk == "L" else self.triUb
                        S.tt(PT[:, ci, :].rearrange("p (g t) -> p g t", g=2),
                             PT[:, ci, :].rearrange("p (g t) -> p g t", g=2),
                             mt.unsqueeze(1).to_broadcast([128, 2, 128]), ALU.mult, eng="pool")
                for g in range(2):
                    h = kvh * 2 + g
                    for ci, (kt, msk) in enumerate(chunks):
                        S.mm(pso[:, h * 65:(h + 1) * 65], PT[:, ci, g * 128:(g + 1) * 128], V[:, kt, kvh, :],
                             start=(ci == 0), stop=(ci == nch - 1))
            po = pso[:, 0:260].rearrange("p (h e) -> p h e", e=65)
            S.tt(den, po[:, :, 64], self.sink_bc, ALU.add)
            S.recip(den, den)
            S.tt(Y.rearrange("p (h e) -> p h e", e=64), po[:, :, 0:64],
                 den.unsqueeze(2).to_broadcast([128, 4, 64]), ALU.mult)
            S.dma(d["Y"][s0:s0 + 128, 512:768], Y)

    def mix_B(self, l):
        S, i, d = self.S, self.i, self.d
        T, S_, NT, NLT = self.T, self.S_, self.NT, self.NLT
        ctx_out = l < self.L - 1
        half = self.half and (l == self.L - 1)
        ar = self.ar
        ar.reset()
        KT = ar.alloc([96, 4, S_], BF16, parts=96)
        for h in range(4):
            for c0 in range(0, S_, 4096):
                c1 = min(S_, c0 + 4096)
                S.dma(KT[:, h, c0:c1], d["B_KT"][:, h, c0:c1])
        V = ar.alloc([128, NT, 4, 65], BF16)
        for t0 in range(0, NT, 16):
            t1 = min(NT, t0 + 16)
            S.dma(V[:, t0:t1, :, :], d["B_V"][t0 * 128:t1 * 128, :, :].rearrange("(t p) h e -> p t h e", p=128))
        QTr = [ar.alloc([96, 4, 512], BF16, parts=96) for _ in range(2)]
        NR = 4
        PTr = [ar.alloc([128, 512], BF16) for _ in range(NR)]
        OTr = [ar.alloc([65, 512], parts=65) for _ in range(2)]
        recr = [ar.alloc([128, 4]) for _ in range(2)]
        Yt = [ar.alloc([128, 4, 256], BF16) for _ in range(2)]
        PS = self.PS
        scale = 96 ** -0.5
        Tq = T // 2 if half else T
        qbs = [(CT + q0, min(512, Tq - q0), NT) for q0 in range(0, Tq, 512)]
        if ctx_out:
            qbs.append((0, CT, 2))
        items = []
        for qi, (s0, n, nkc) in enumerate(qbs):
            for h in range(4):
                for kc in range(nkc):
                    items.append((qi, h, kc))
        LA = 3
        loaded = set()

        def load_q(qi):
            if qi in loaded or qi >= len(qbs):
                return
            loaded.add(qi)
            s0, n, nkc = qbs[qi]
            S.dma(QTr[qi % 2][:, :, 0:n], d["B_QT"][:, :, s0:s0 + n])

        def QK(j):
            qi, h, kc = items[j]
            s0, n, nkc = qbs[qi]
            load_q(qi)
            S.mm(PS[j % NR][:, 0:n], KT[:, h, kc * 128:(kc + 1) * 128], QTr[qi % 2][:, h, 0:n])

        deferred = []
        nfin = [0]

        def fin_pe(qi, h, fi):
            s0, n, nkc = qbs[qi]
            nsub = n // 128
            OT = OTr[fi % 2]
            pst = PS[6 + (fi % 2)]
            rec = recr[fi % 2]
            Y = Yt[qi % 2]
            for sub in range(nsub):
                S.transpose(pst[:, sub * 65:(sub + 1) * 65], OT[:, sub * 128:(sub + 1) * 128], self.ident[0:65, 0:65])
            pt = pst[:, 0:nsub * 65].rearrange("p (s e) -> p s e", e=65)
            S.recip(rec[:, 0:nsub], pt[:, :, 64])
            S.tt(Y[:, 0:nsub, h * 64:(h + 1) * 64], pt[:, :, 0:64],
                 rec[:, 0:nsub].unsqueeze(2).to_broadcast([128, nsub, 64]), ALU.mult)
            if h == 3:
                S.dma(d["Y"][s0:s0 + n, 256:512].rearrange("(s p) e -> p s e", p=128), Y[:, 0:nsub, :])

        for j in range(min(LA, len(items))):
            QK(j)
        for j, (qi, h, kc) in enumerate(items):
            s0, n, nkc = qbs[qi]
            if j + LA < len(items):
                QK(j + LA)
            while deferred and deferred[0][0] <= j:
                deferred.pop(0)[1]()
            pso = PS[4 + (h % 2)]
            PT = PTr[j % NR]
            S.act(PT[:, 0:n], PS[j % NR][:, 0:n], AF.Exp, scale=scale)
            S.mm(pso[0:65, 0:n], V[:, kc, h, :], PT[:, 0:n], start=(kc == 0), stop=(kc == nkc - 1))
            if kc == nkc - 1:
                fi = nfin[0]
                nfin[0] += 1
                S.copy(OTr[fi % 2][:, 0:n], pso[0:65, 0:n])
                deferred.append((j + 3, lambda qi=qi, h=h, fi=fi: fin_pe(qi, h, fi)))
        while deferred:
            deferred.pop(0)[1]()

    def mix_B_old(self, l):
        S, i, d = self.S, self.i, self.d
        T, S_, NT, NLT = self.T, self.S_, self.NT, self.NLT
        ctx_out = l < self.L - 1
        ar = self.ar
        ar.reset()
        KT = ar.alloc([96, 4, S_], BF16, parts=96)
        for h in range(4):
            for c0 in range(0, S_, 4096):
                c1 = min(S_, c0 + 4096)
                S.dma(KT[:, h, c0:c1], d["B_KT"][:, h, c0:c1])
        V = ar.alloc([128, NT, 4, 65], BF16)
        for t0 in range(0, NT, 16):
            t1 = min(NT, t0 + 16)
            S.dma(V[:, t0:t1, :, :], d["B_V"][t0 * 128:t1 * 128, :, :].rearrange("(t p) h e -> p t h e", p=128))
        QTr = [ar.alloc([96, 4, 512], BF16, parts=96) for _ in range(2)]
        PTr = [ar.alloc([128, 512], BF16) for _ in range(3)]
        OT = ar.alloc([65, 512], parts=65)
        rec = ar.alloc([128, 4])
        Yt = [ar.alloc([128, 4, 256], BF16) for _ in range(2)]
        PS = self.PS
        scale = 96 ** -0.5
        Tq = T // 2 if (self.half and l == self.L - 1) else T
        qbs = [(CT + q0, min(512, Tq - q0), NT) for q0 in range(0, Tq, 512)]
        if ctx_out:
            qbs.append((0, CT, 2))
        k = 0
        for qi, (s0, n, nkc) in enumerate(qbs):
            QT = QTr[qi % 2]
            S.dma(QT[:, :, 0:n], d["B_QT"][:, :, s0:s0 + n])
            Y = Yt[qi % 2]
            nsub = n // 128
            for h in range(4):
                pso = PS[4 + (h % 2)]
                for kc in range(nkc):
                    pss = PS[k % 3]
                    PT = PTr[k % 3]
                    k += 1
                    S.mm(pss[:, 0:n], KT[:, h, kc * 128:(kc + 1) * 128], QT[:, h, 0:n])
                    S.act(PT[:, 0:n], pss[:, 0:n], AF.Exp, scale=scale)
                    S.mm(pso[0:65, 0:n], V[:, kc, h, :], PT[:, 0:n], start=(kc == 0), stop=(kc == nkc - 1))
                S.copy(OT[:, 0:n], pso[0:65, 0:n])
                pst = PS[6 + (h % 2)]
                for sub in range(nsub):
                    S.transpose(pst[:, sub * 65:(sub + 1) * 65], OT[:, sub * 128:(sub + 1) * 128], self.ident[0:65, 0:65])
                pt = pst[:, 0:nsub * 65].rearrange("p (s e) -> p s e", e=65)
                S.recip(rec[:, 0:nsub], pt[:, :, 64])
                S.tt(Y[:, 0:nsub, h * 64:(h + 1) * 64], pt[:, :, 0:64],
                     rec[:, 0:nsub].unsqueeze(2).to_broadcast([128, nsub, 64]), ALU.mult)
            S.dma(d["Y"][s0:s0 + n, 256:512].rearrange("(s p) e -> p s e", p=128), Y[:, 0:nsub, :])

    def mix_A(self, l):
        S, i, d = self.S, self.i, self.d
        T, S_, NT, NLT = self.T, self.S_, self.NT, self.NLT
        ctx_out = l < self.L - 1
        ar = self.ar
        ar.reset()
        PS = self.PS
        def ring(shape, dt=F32, parts=128, n=2):
            return [ar.alloc(shape, dt, parts=parts) for _ in range(n)]
        qTr = ring([64, 4, 128], BF16, 64)
        kTr = ring([64, 4, 128], BF16, 64)
        ktr = ring([128, 256], BF16)
        vr = ring([128, 4, 65], BF16)
        gr = ring([128, 16])
        sor = ring([128, 256], BF16)
        hfr = ring([128, 256])
        sm = ar.alloc([128, 12])
        e12 = ar.alloc([128, 12])
        ib = ar.alloc([128, 4])
        wv = ar.alloc([128, 4])
        ET = ar.alloc([128, 4, 128])
        PTb = ar.alloc([128, 4, 128], BF16)
        INTs = ar.alloc([128, 4, 65])
        NUM = ar.alloc([128, 4, 65])
        den = ar.alloc([128, 4])
        hh = ar.alloc([128, 4, 64])
        hsq = ar.alloc([128, 256])
        ss4 = ar.alloc([128, 4])
        Yt = ring([128, 256], BF16)
        vw = ar.alloc([128, 4, 65], BF16)
        C = ar.alloc([64, 4, 65], parts=64)
        Ctmp = ar.alloc([64, 4, 65], parts=64)
        Cb = ar.alloc([64, 4, 65], BF16, parts=64)
        halfL = self.half and (l == self.L - 1)
        nq = NLT // 2 if halfL else NLT
        order_f = list(range(2 + nq))
        order_b = [1, 0] + list(range(NT - 1, 1, -1))
        k = 0
        for dr, order in ((0, order_f), (1, order_b)):
            S.memset(C, 0.0)
            S.memset(Cb, 0.0)
            io = 8 * dr
            cumT = self.triU if dr == 0 else self.triL
            revT = self.triLs if dr == 0 else self.triUs
            for ti in order:
                s0 = ti * 128
                lat = ti >= 2
                with_out = (lat and (ti - 2) < nq) or ((not lat) and ctx_out)
                kk = k % 2
                k += 1
                qT, kT, kt, v, g16 = qTr[kk], kTr[kk], ktr[kk], vr[kk], gr[kk]
                S.dma(kt, d["A_k"][s0:s0 + 128, :])
                S.dma(v, d["A_v"][s0:s0 + 128, :, :])
                S.dma(g16, d["A_g"][s0:s0 + 128, :])
                i4 = g16[:, io:io + 4]
                lf4 = g16[:, io + 4:io + 8]
                if with_out:
                    S.dma(qT, d["A_qT"][:, :, s0:s0 + 128])
                    S.dma(kT, d["A_kT"][:, :, s0:s0 + 128])
                S.mm(PS[0][:, 0:4], cumT, lf4)
                S.mm(PS[0][:, 4:8], revT, lf4)
                S.mm(PS[0][:, 8:12], self.ones, lf4)
                S.copy(sm, PS[0][:, 0:12])
                S.act(e12, sm, AF.Exp)
                S.tt(wv, sm[:, 4:8], i4, ALU.add)
                S.act(wv, wv, AF.Exp)
                if with_out:
                    S.tt(ib, i4, sm[:, 0:4], ALU.subtract)
                    for h in range(4):
                        S.mm(PS[1][:, h * 128:(h + 1) * 128], lf4[:, h:h + 1].to_broadcast([128, 128]), cumT)
                        S.mm(PS[2][:, h * 128:(h + 1) * 128], kT[:, h, :], qT[:, h, :])
                    for h in range(4):
                        S.act(ET[:, h, :], PS[1][:, h * 128:(h + 1) * 128], AF.Exp, bias=ib[:, h:h + 1])
                    S.tt(ET, ET, cumT.unsqueeze(1).to_broadcast([128, 4, 128]), ALU.mult, eng="pool")
                    S.tt(PTb, ET, PS[2][:, :].rearrange("p (h t) -> p h t", h=4), ALU.mult)
                    for h in range(4):
                        S.mm(PS[3][:, h * 65:(h + 1) * 65], PTb[:, h, :], v[:, h, :])
                        S.mm(PS[4][:, h * 65:(h + 1) * 65], qT[:, h, :], Cb[:, h, :])
                    for h in range(4):
                        S.act(INTs[:, h, :], PS[4][:, h * 65:(h + 1) * 65], AF.Copy, scale=e12[:, h:h + 1])
                    S.tt(NUM, INTs, PS[3][:, 0:260].rearrange("p (h e) -> p h e", e=65), ALU.add)
                    S.stt(den, NUM[:, :, 64], -1.0, NUM[:, :, 64], ALU.mult, ALU.max)
                    S.ts(den, den, 1.0, None, ALU.max)
                    S.recip(den, den)
                    S.tt(hh, NUM[:, :, 0:64], den.unsqueeze(2).to_broadcast([128, 4, 64]), ALU.mult)
                    hhf = hh.rearrange("p h e -> p (h e)")
                    if dr == 0:
                        S.dma(d["A_hf"][s0:s0 + 128, :], hhf)
                    else:
                        hf = hfr[kk]
                        so = sor[kk]
                        S.dma(hf, d["A_hf"][s0:s0 + 128, :])
                        S.dma(so, d["A_so"][s0:s0 + 128, :])
                        S.tt(hhf, hhf, hf, ALU.add)
                        S.tt(hsq, hhf, hhf, ALU.mult, eng="pool")
                        S.reduce(ss4, hsq.rearrange("p (h e) -> p h e", e=64), ALU.add)
                        self.rstd(ss4, 1.0 / 64)
                        S.tt(hh, hh, ss4.unsqueeze(2).to_broadcast([128, 4, 64]), ALU.mult)
                        S.tt(hhf, hhf, self.mlon_bc, ALU.mult)
                        Y = Yt[kk]
                        S.tt(Y, hhf, so, ALU.mult)
                        S.dma(d["Y"][s0:s0 + 128, 0:256], Y)
                S.tt(vw, v, wv.unsqueeze(2).to_broadcast([128, 4, 65]), ALU.mult)
                for h in range(4):
                    S.mm(PS[5][0:64, h * 65:(h + 1) * 65], kt[:, h * 64:(h + 1) * 64], vw[:, h, :])
                S.tt(Ctmp, C, e12[0:64, 8:12].unsqueeze(2).to_broadcast([64, 4, 65]), ALU.mult)
                S.tt(C, Ctmp, PS[5][0:64, 0:260].rearrange("p (h e) -> p h e", e=65), ALU.add)
                S.copy(Cb, C, eng="act")

    def P3(self, l):
        S, i, d = self.S, self.i, self.d
        T, S_, NT, NLT = self.T, self.S_, self.NT, self.NLT
        ctx_out = l < self.L - 1
        moe = (l % 2 == 1)
        ar = self.ar
        ar.reset()
        PS = self.PS
        wout = ar.alloc([128, 8, D], BF16)
        NM = 512
        x1 = ar.alloc([128, 4, D])
        acc = ar.alloc([128, 4, D])
        hT2 = ar.alloc([128, 8, NM], BF16)
        yab = [ar.alloc([128, 768], BF16) for _ in range(2)]
        yT = [ar.alloc([128, 8, 128], BF16) for _ in range(2)]
        xin = [ar.alloc([128, D]) for _ in range(2)]
        otmp = ar.alloc([128, D])
        tmp = {"junk": ar.alloc([128, D]), "ssq": ar.alloc([128, 1]), "xn": ar.alloc([128, D], BF16)}
        stgs = [xin[0], xin[1], otmp, tmp["junk"]]
        for c in range(8):
            S.dma(stgs[c % 4], i["w_out"][l, c * 128:(c + 1) * 128, :])
            S.copy(wout[:, c, :], stgs[c % 4], eng="pool")
        if moe:
            tmp["xn32"] = ar.alloc([128, D])
            hT32 = ar.alloc([128, 8, 128])
            rout = ar.alloc([128, 8, NEXP])
            S.dma(rout, i["moe_router"][0].rearrange("(c p) e -> p c e", p=128))
            rb = ar.alloc([128, NEXP])
            S.dma(rb, i["moe_router_b"][0:1, :].to_broadcast([128, NEXP]))
            lg = ar.alloc([128, NEXP])
            m8 = ar.alloc([128, 8])
            gts = ar.alloc([128, 2])
            eq = ar.alloc([128, NEXP])
            comb = ar.alloc([128, 4, NEXP])
            nexp, npc, pw, nfc = NEXP, 7, 512, 28
        else:
            nexp, npc, pw, nfc = 1, 11, 256, 22
        actT = ar.alloc([128, nfc, NM], BF16)
        sg = [ar.alloc([128, NM]) for _ in range(2)]
        w13r = [ar.alloc([128, 8, 2, pw], BF16) for _ in range(2)]
        w2r = [ar.alloc([128, 4, 512], BF16) for _ in range(3)]
        xo = [ar.alloc([128, 512]) for _ in range(2)]
        mts = []
        if ctx_out:
            mts.append((0, CT))
        halfL = self.half and (l == self.L - 1)
        send = CT + (T // 2 if halfL else T)
        for q0 in range(CT, send, NM):
            mts.append((q0, min(NM, send - q0)))
        kw13 = 0
        kw2 = 0
        ksg = 0
        kx = 0
        ksub = 0
        for (m0, n) in mts:
            nsub = n // 128
            role = 1 if m0 < CT else 0
            g1bc = self.bc[:, role, :]
            g2bc = self.bc[:, 2 + role, :]
            b0 = 16 + role * 48
            gm2, sh2 = self.vec[:, b0 + 40:b0 + 48], self.vec[:, b0 + 24:b0 + 32]
            for sub in range(nsub):
                s0 = m0 + sub * 128
                ya = yab[ksub % 2]
                yt = yT[ksub % 2]
                xt = xin[ksub % 2]
                ksub += 1
                S.dma(ya, d["Y"][s0:s0 + 128, :])
                S.dma(yt[:, 6:8, :], d["YTD"][:, :, s0:s0 + 128])
                if l == 0:
                    src = i["ctx"][s0:s0 + 128, :] if s0 < CT else i["x"][s0 - CT:s0 - CT + 128, :]
                else:
                    src = d["X1"][s0:s0 + 128, :]
                S.dma(xt, src)
                pb = self.PSb(7)
                for c in range(6):
                    S.transpose(pb[:, c * 128:(c + 1) * 128], ya[:, c * 128:(c + 1) * 128], self.identb)
                S.copy(yt[:, 0:6, :], pb[:, 0:768].rearrange("p (c t) -> p c t", c=6), eng="act")
                for half in range(2):
                    for c in range(8):
                        S.mm(PS[half][:, :], yt[:, c, :], wout[:, c, half * 512:(half + 1) * 512],
                             start=(c == 0), stop=(c == 7), signal=(c == 7))
                for half in range(2):
                    hs = slice(half * 512, (half + 1) * 512)
                    S.tt(otmp[:, hs], PS[half][:, :], g1bc[:, hs], ALU.mult)
                    S.tt(x1[:, sub, hs], otmp[:, hs], xt[:, hs], ALU.add)
                if moe:
                    self.norm_mod_T(x1[:, sub, :], gm2, sh2, hT2[:, :, sub * 128:(sub + 1) * 128], tmp, None,
                                    f32_out=hT32, psf=[PS[2], PS[3]])
                    for c in range(8):
                        S.mm(PS[4][:, 0:NEXP], hT32[:, c, :], rout[:, c, :], start=(c == 0), stop=(c == 7))
                    S.tt(lg, PS[4][:, 0:NEXP], rb, ALU.add)
                    S.add("dve", lambda e, o_=m8, i_=lg: e.max(out=o_, in_=i_), reads=[lg], writes=[m8])
                    S.tt(gts[:, 0:1], m8[:, 0:1], m8[:, 1:2], ALU.subtract)
                    S.act(gts[:, 0:1], gts[:, 0:1], AF.Sigmoid)
                    S.ts(gts[:, 1:2], gts[:, 0:1], -1.0, 1.0, ALU.mult, ALU.add)
                    S.ts(comb[:, sub, :], lg, m8[:, 0:1], gts[:, 0:1], ALU.is_equal, ALU.mult)
                    S.ts(eq, lg, m8[:, 1:2], gts[:, 1:2], ALU.is_equal, ALU.mult)
                    S.tt(comb[:, sub, :], comb[:, sub, :], eq, ALU.add)
                else:
                    self.norm_mod_T(x1[:, sub, :], gm2, sh2, hT2[:, :, sub * 128:(sub + 1) * 128], tmp, self.PSb(6))
            for e in range(nexp):
                for pc in range(npc):
                    w13 = w13r[kw13 % 2]
                    kw13 += 1
                    if moe:
                        S.dma(w13, d["W13e"][e, pc])
                    else:
                        S.dma(w13, d["W13d"][pc])
                    for f in range(pw // 128):
                        fc = pc * (pw // 128) + f
                        pa = PS[2 + (fc % 2) * 2]
                        pg = PS[3 + (fc % 2) * 2]
                        for c in range(8):
                            S.mm(pa[:, 0:n], w13[:, c, 0, f * 128:(f + 1) * 128], hT2[:, c, 0:n], start=(c == 0), stop=(c == 7), signal=(c == 7))
                        for c in range(8):
                            S.mm(pg[:, 0:n], w13[:, c, 1, f * 128:(f + 1) * 128], hT2[:, c, 0:n], start=(c == 0), stop=(c == 7), signal=(c == 7))
                        sgt = sg[ksg % 2]
                        ksg += 1
                        S.act(sgt[:, 0:n], pg[:, 0:n], AF.Silu)
                        S.tt(actT[:, fc, 0:n], sgt[:, 0:n], pa[:, 0:n], ALU.mult)
                for half in range(2):
                    for g4 in range(0, nfc, 4):
                        ng = min(4, nfc - g4)
                        w2 = w2r[kw2 % 3]
                        kw2 += 1
                        if moe:
                            srcw = d["W2e"][e, g4 * 128:(g4 + ng) * 128, half * 512:(half + 1) * 512]
                        else:
                            srcw = d["W2d"][g4 * 128:(g4 + ng) * 128, half * 512:(half + 1) * 512]
                        S.dma(w2[:, 0:ng, :], srcw.rearrange("(f p) n -> p f n", p=128))
                        for sub in range(nsub):
                            for f in range(ng):
                                fc = g4 + f
                                S.mm(PS[4 + sub][:, :], actT[:, fc, sub * 128:(sub + 1) * 128], w2[:, f, :],
                                     start=(fc == 0), stop=(fc == nfc - 1), signal=(f == ng - 1))
                    hs = slice(half * 512, (half + 1) * 512)
                    for sub in range(nsub):
                        if moe:
                            if e == 0:
                                S.ts(acc[:, sub, hs], PS[4 + sub][:, :], comb[:, sub, e:e + 1], None, ALU.mult)
                            else:
                                S.stt(acc[:, sub, hs], PS[4 + sub][:, :], comb[:, sub, e:e + 1], acc[:, sub, hs],
                                      ALU.mult, ALU.add)
                        else:
                            S.copy(acc[:, sub, hs], PS[4 + sub][:, :], eng="act")
            for sub in range(nsub):
                s0 = m0 + sub * 128
                for half in range(2):
                    hs = slice(half * 512, (half + 1) * 512)
                    xot = xo[kx % 2]
                    kx += 1
                    S.tt(xot, acc[:, sub, hs], g2bc[:, hs], ALU.mult, eng="pool")
                    S.tt(xot, xot, x1[:, sub, hs], ALU.add, eng="pool")
                    if l == self.L - 1:
                        S.dma(self.out[s0 - CT:s0 - CT + 128, hs], xot, is_output=True)
                    else:
                        S.dma(d["X1"][s0:s0 + 128, hs], xot)

    def build(self):
        self.declare()
        st = self.stages
        self.prep()
        for l in range(self.L):
            if st is not None and l not in st:
                continue
            sl = st[l] if st is not None else "P1 A B C D P3"
            self.layer_setup(l)
            if "P1" in sl:
                self.P1(l)
            if l == 0 and self.L > 1 and (st is None or 1 in st):
                self.prep_moe()
            if "D" in sl.split():
                self.mix_D(l)
            if "C" in sl.split():
                self.mix_C(l)
            if "A" in sl.split():
                self.mix_A(l)
            if "B" in sl.split():
                if PIPE_MLA:
                    self.mix_B(l)
                else:
                    self.mix_B_old(l)
            if "P3" in sl:
                self.P3(l)
        self.S.emit()
        self.st.close()
        return self.nc


def _rope_tables(T):
    t = np.arange(T)
    rows = (t // 64).astype(np.float32)
    cols = (t % 64).astype(np.float32)

    def tab(nf):
        freqs = (10000.0 ** (-np.arange(nf, dtype=np.float32) / nf)).astype(np.float32)
        ar_ = rows[:, None] * freqs[None, :]
        ac_ = cols[:, None] * freqs[None, :]
        cr, sr, cc, sc = np.cos(ar_), np.sin(ar_), np.cos(ac_), np.sin(ac_)
        cos = np.concatenate([cr, cr, cc, cc], 1)
        sin = np.concatenate([-sr, sr, -sc, sc], 1)
        return np.stack([cos, sin], 1).astype(np.float32)

    return tab(8), tab(16)


def _consts():
    k = np.arange(128)[:, None]
    t = np.arange(128)[None, :]
    mats = [(k == t), (k <= t), (k >= t), (k < t), (k > t), np.ones((128, 128), bool)]
    return np.concatenate([m.astype(np.float32) for m in mats], 1)


def _host_inputs(inputs, b, T, flip=False):
    f = lambda a: np.ascontiguousarray(np.asarray(a, dtype=np.float32))
    m = {}
    x = np.asarray(inputs["x"])[b, :T]
    ctx = np.asarray(inputs["ctx"])[b]
    if flip:
        x = x[::-1]
        ctx = ctx[::-1]
    m["x"] = f(x)
    m["ctx"] = f(ctx)
    m["cvec"] = f(np.stack([np.asarray(inputs["c"][b]), np.asarray(inputs["c_ctx"])]))
    for k in ("ada_w", "ada_b", "norm_mix", "norm_ffn", "w_out", "ml_out_norm", "mla_q_norm",
              "mla_kv_norm", "mla_q_gain", "mla_k_gain", "sw_q_gain", "sw_k_gain", "sw_sink",
              "lru_conv_b", "ffn_w13", "ffn_w2", "moe_router", "moe_router_b", "moe_w13", "moe_w2"):
        m[k] = f(inputs[k])
    w_in = np.asarray(inputs["w_in"], dtype=np.float32)
    gb = np.asarray(inputs["ml_gate_b"], dtype=np.float32)
    if flip:
        w_in = np.concatenate([w_in[:, :, :1024], w_in[:, :, 1032:1040], w_in[:, :, 1024:1032], w_in[:, :, 1040:]], 2)
        gb = np.concatenate([gb[:, 8:16], gb[:, 0:8]], 1)
    m["w_in"] = f(w_in)
    m["ml_gate_b"] = f(gb)
    for k in ("lru_wa", "lru_ba", "lru_wx", "lru_bx", "lru_lam"):
        a = np.asarray(inputs[k], dtype=np.float32)
        m[k] = f(a[:, ::-1] if flip else a)
    cw = np.asarray(inputs["lru_conv_w"], dtype=np.float32)
    z = np.zeros_like(cw[:, :1])
    m["lru_conv_w"] = f(np.concatenate([z, cw[:, ::-1]], 1) if flip else np.concatenate([cw, z], 1))
    uq = np.asarray(inputs["mla_w_uq"], dtype=np.float32)
    L = uq.shape[0]
    uq4 = uq.reshape(L, 192, 4, 96)
    m["mla_w_uq"] = np.ascontiguousarray(
        np.concatenate([uq4[:, :, :, :64].reshape(L, 192, 256), uq4[:, :, :, 64:].reshape(L, 192, 128)], 2))
    ukv = np.asarray(inputs["mla_w_ukv"], dtype=np.float32).reshape(L, 128, 4, 128)
    m["mla_w_ukv"] = np.ascontiguousarray(
        np.concatenate([ukv[:, :, :, :64].reshape(L, 128, 256), ukv[:, :, :, 64:].reshape(L, 128, 256)], 2))
    m["cst"] = _consts()
    rb, rc = _rope_tables(T)
    m["ropeB"] = f(rb[::-1] if flip else rb)
    m["ropeC"] = f(rc[::-1] if flip else rc)
    return m


_NC_CACHE = {}


HALF = True


def kernel(**inputs):
    x = np.asarray(inputs["x"])
    B, T = int(x.shape[0]), int(x.shape[1])
    key = (T, HALF)
    if key not in _NC_CACHE:
        _NC_CACHE[key] = MK(T, half=HALF).build()
    nc = _NC_CACHE[key]
    ncores = 8
    out = np.empty((B, T, D), np.float32)
    if HALF:
        assert 2 * B == ncores
        in_maps = [_host_inputs(inputs, c // 2, T, flip=bool(c % 2)) for c in range(ncores)]
        res = run_bass_kernel_spmd(nc, in_maps, core_ids=list(range(ncores)))
        for c in range(ncores):
            o = np.asarray(res.results[c]["out"], dtype=np.float32)
            if c % 2 == 0:
                out[c // 2, :T // 2] = o
            else:
                out[c // 2, T // 2:] = o[::-1]
    else:
        base = [_host_inputs(inputs, b, T) for b in range(B)]
        in_maps = [base[c % B] for c in range(ncores)]
        res = run_bass_kernel_spmd(nc, in_maps, core_ids=list(range(ncores)))
        for b in range(B):
            out[b] = np.asarray(res.results[b]["out"], dtype=np.float32)
    return out
```
